# Optimizing a Trainium2 kernel written in Bass

```python
import math, functools
import jax, jax.numpy as jnp
from jax import lax
import numpy as np

D_MODEL = 1024
BATCH = 8
SEQ = 4096
DEPTH = 2

GRID_W = 64
CTX_LEN = 256

SSM_WIDTH = 512
SSM_GROUP = 16
SSM_GROUPS = SSM_WIDTH // SSM_GROUP
SSM_STATE = 64
DT_MIN = 1e-3
DT_MAX = 1e-1

HEAD_DIM = 64
GQA_HEADS = 8
GQA_KV_HEADS = 2
GQA_GROUP = GQA_HEADS // GQA_KV_HEADS
Q_BLOCK = 128
ROPE_BASE = 10000.0
NA_HEADS = 8
NA_WIN_R = 8
NA_WIN_C = 16
ATTN_SCALE = HEAD_DIM ** -0.5

GQA_Q_W = GQA_HEADS * HEAD_DIM
GQA_KV_W = GQA_KV_HEADS * HEAD_DIM
NA_W = NA_HEADS * HEAD_DIM
N_BRANCHES = 3

KV_COLS = SSM_WIDTH + 2 * GQA_KV_W + 2 * NA_W
IN_COLS = KV_COLS + GQA_Q_W + NA_W + N_BRANCHES * D_MODEL
IN_SPLITS = [
    SSM_WIDTH,
    SSM_WIDTH + GQA_KV_W,
    SSM_WIDTH + 2 * GQA_KV_W,
    SSM_WIDTH + 2 * GQA_KV_W + NA_W,
    KV_COLS,
    KV_COLS + GQA_Q_W,
    KV_COLS + GQA_Q_W + NA_W,
]

FFN_DIM = 2816
N_EXPERTS = 8
TOP_K = 2
EXPERT_DIM = 3584
N_DENSE = (DEPTH + 1) // 2
N_MOE = DEPTH // 2

NORM_EPS = 1e-6

kernel_name = "hybrid_s5_gqa_natten_moe_dit"


def rms_norm(x, g):
    xf = x.astype(jnp.float32)
    y = xf * lax.rsqrt(jnp.mean(xf * xf, axis=-1, keepdims=True) + NORM_EPS)
    return (y * g.astype(jnp.float32)).astype(x.dtype)


def modulate(h, shift, scale):
    return h * (1.0 + scale) + shift


def adaln(cvec, w, b, n_chunks):
    m = jax.nn.silu(cvec) @ w[:, :n_chunks * D_MODEL] + b[:n_chunks * D_MODEL]
    return jnp.split(m, n_chunks, axis=-1)


def axial_rope(n_tokens, dtype):
    t = jnp.arange(n_tokens)
    pos = jnp.stack([t // GRID_W, t % GRID_W], axis=-1).astype(jnp.float32)
    half = HEAD_DIM // 2
    inv = 1.0 / (ROPE_BASE ** (jnp.arange(0, half, 2, dtype=jnp.float32) / half))
    ang = pos[:, :, None] * inv
    ang = jnp.concatenate([ang, ang], axis=-1).reshape(n_tokens, HEAD_DIM)
    return jnp.cos(ang).astype(dtype), jnp.sin(ang).astype(dtype)


def apply_rope(x, cos, sin):
    xs = x.reshape(x.shape[:-1] + (2, 2, HEAD_DIM // 4))
    rot = jnp.stack([-xs[..., 1, :], xs[..., 0, :]], axis=-2).reshape(x.shape)
    return x * cos[:, None, :] + rot * sin[:, None, :]


def s5_discretize(a_re, a_im, b_re, b_im, log_dt):
    lam = lax.complex(a_re.astype(jnp.float32), a_im.astype(jnp.float32))
    dt = jnp.exp(log_dt.astype(jnp.float32))[:, None]
    a_bar = jnp.exp(lam * dt)
    b = lax.complex(b_re.astype(jnp.float32), b_im.astype(jnp.float32))
    b_bar = ((a_bar - 1.0) / lam)[..., None] * b
    return a_bar, b_bar


def diag_scan(a_bar, bu, s0, reverse):
    if s0 is not None:
        idx = -1 if reverse else 0
        bu = bu.at[:, idx].add(a_bar * s0)
    a = jnp.broadcast_to(a_bar, bu.shape)

    def combine(e1, e2):
        a1, b1 = e1
        a2, b2 = e2
        return a1 * a2, a2 * b1 + b2

    _, s = lax.associative_scan(combine, (a, bu), reverse=reverse, axis=1)
    return s


def s5_glu(y, w, b):
    y = jax.nn.gelu(y)
    return y * jax.nn.sigmoid(y @ w + b)


def s5_bidirectional(u, uc, a_re, a_im, b_re, b_im, c_re, c_im, log_dt, d_skip, glu_w, glu_b, need_ctx):
    bsz, n_lat, _ = u.shape
    n_ctx = uc.shape[1]
    dtype = u.dtype
    uf = u.astype(jnp.float32)
    ucf = uc.astype(jnp.float32)
    ug = uf.reshape(bsz, n_lat, SSM_GROUPS, SSM_GROUP)
    ucg = ucf.reshape(bsz, n_ctx, SSM_GROUPS, SSM_GROUP)
    d = d_skip.astype(jnp.float32)
    y = uf * d
    yc = ucf * d if need_ctx else None
    for direction in range(2):
        rev = direction == 1
        a_bar, b_bar = s5_discretize(a_re[direction], a_im[direction], b_re[direction],
                                     b_im[direction], log_dt[direction])
        c_mat = lax.complex(c_re[direction].astype(jnp.float32), c_im[direction].astype(jnp.float32))
        s_ctx = diag_scan(a_bar, jnp.einsum('bcgh,gph->bcgp', ucg, b_bar), None, rev)
        s_init = s_ctx[:, 0] if rev else s_ctx[:, -1]
        s_lat = diag_scan(a_bar, jnp.einsum('blgh,gph->blgp', ug, b_bar), s_init, rev)
        y = y + jnp.real(jnp.einsum('blgp,ghp->blgh', s_lat, c_mat)).reshape(bsz, n_lat, SSM_WIDTH)
        if need_ctx:
            yc = yc + jnp.real(jnp.einsum('bcgp,ghp->bcgh', s_ctx, c_mat)).reshape(bsz, n_ctx, SSM_WIDTH)
    y_out = s5_glu(y.astype(dtype), glu_w, glu_b)
    yc_out = s5_glu(yc.astype(dtype), glu_w, glu_b) if need_ctx else None
    return y_out, yc_out


def gqa_branch(q, k, v, qc, kc, vc, q_g, k_g, cos, sin):
    bsz, n_lat, _ = q.shape
    n_ctx = kc.shape[1]
    q = apply_rope(rms_norm(q.reshape(bsz, n_lat, GQA_HEADS, HEAD_DIM), q_g), cos, sin)
    k = apply_rope(rms_norm(k.reshape(bsz, n_lat, GQA_KV_HEADS, HEAD_DIM), k_g), cos, sin)
    v = v.reshape(bsz, n_lat, GQA_KV_HEADS, HEAD_DIM)
    kc = rms_norm(kc.reshape(bsz, n_ctx, GQA_KV_HEADS, HEAD_DIM), k_g)
    vc = vc.reshape(bsz, n_ctx, GQA_KV_HEADS, HEAD_DIM)
    k_all = jnp.concatenate([k, kc], axis=1)
    v_all = jnp.concatenate([v, vc], axis=1)
    n_blk = n_lat // Q_BLOCK
    qb = q.reshape(bsz, n_blk, Q_BLOCK, GQA_KV_HEADS, GQA_GROUP, HEAD_DIM).transpose(1, 0, 2, 3, 4, 5)

    def block(qi):
        s = jnp.einsum('bqkgd,bskd->bkgqs', qi, k_all).astype(jnp.float32) * ATTN_SCALE
        p = jax.nn.softmax(s, axis=-1).astype(v_all.dtype)
        return jnp.einsum('bkgqs,bskd->bqkgd', p, v_all)

    o = lax.map(block, qb)
    o = o.transpose(1, 0, 2, 3, 4, 5).reshape(bsz, n_lat, GQA_Q_W)
    oc = None
    if qc is not None:
        qc = rms_norm(qc.reshape(bsz, n_ctx, GQA_KV_HEADS, GQA_GROUP, HEAD_DIM), q_g)
        s = jnp.einsum('bqkgd,bskd->bkgqs', qc, kc).astype(jnp.float32) * ATTN_SCALE
        p = jax.nn.softmax(s, axis=-1).astype(vc.dtype)
        oc = jnp.einsum('bkgqs,bskd->bqkgd', p, vc).reshape(bsz, n_ctx, GQA_Q_W)
    return o, oc


def na_branch(q, k, v, qc, kc, vc, rpb):
    bsz, n_lat, _ = q.shape
    n_ctx = kc.shape[1]
    rows = n_lat // GRID_W
    win_r = min(NA_WIN_R, rows)
    n_win = win_r * NA_WIN_C
    qg = q.reshape(bsz, rows, GRID_W, NA_HEADS, HEAD_DIM)
    kg = k.reshape(bsz, rows, GRID_W, NA_HEADS, HEAD_DIM)
    vg = v.reshape(bsz, rows, GRID_W, NA_HEADS, HEAD_DIM)
    kc = kc.reshape(bsz, n_ctx, NA_HEADS, HEAD_DIM)
    vc = vc.reshape(bsz, n_ctx, NA_HEADS, HEAD_DIM)
    cols = jnp.arange(GRID_W)
    col_start = jnp.clip(cols - NA_WIN_C // 2, 0, GRID_W - NA_WIN_C)
    col_idx = col_start[:, None] + jnp.arange(NA_WIN_C)
    col_bias_idx = col_idx - cols[:, None] + NA_WIN_C - 1

    def row_block(r):
        rs = jnp.clip(r - win_r // 2, 0, rows - win_r)
        q_r = lax.dynamic_index_in_dim(qg, r, axis=1, keepdims=False)
        k_win = lax.dynamic_slice_in_dim(kg, rs, win_r, axis=1)[:, :, col_idx]
        v_win = lax.dynamic_slice_in_dim(vg, rs, win_r, axis=1)[:, :, col_idx]
        s_win = jnp.einsum('bqhd,brqkhd->bhqrk', q_r, k_win).astype(jnp.float32) * ATTN_SCALE
        row_bias_idx = rs + jnp.arange(win_r) - r + NA_WIN_R - 1
        bias = rpb[:, row_bias_idx][:, :, col_bias_idx]
        s_win = s_win + bias.transpose(0, 2, 1, 3)[None].astype(jnp.float32)
        s_ctx = jnp.einsum('bqhd,bshd->bhqs', q_r, kc).astype(jnp.float32) * ATTN_SCALE
        s = jnp.concatenate([s_win.reshape(bsz, NA_HEADS, GRID_W, n_win), s_ctx], axis=-1)
        p = jax.nn.softmax(s, axis=-1).astype(v.dtype)
        p_win = p[..., :n_win].reshape(bsz, NA_HEADS, GRID_W, win_r, NA_WIN_C)
        p_ctx = p[..., n_win:]
        return (jnp.einsum('bhqrk,brqkhd->bqhd', p_win, v_win)
                + jnp.einsum('bhqs,bshd->bqhd', p_ctx, vc))

    o = lax.map(row_block, jnp.arange(rows))
    o = o.transpose(1, 0, 2, 3, 4).reshape(bsz, n_lat, NA_W)
    oc = None
    if qc is not None:
        qc = qc.reshape(bsz, n_ctx, NA_HEADS, HEAD_DIM)
        s = jnp.einsum('bqhd,bshd->bhqs', qc, kc).astype(jnp.float32) * ATTN_SCALE
        p = jax.nn.softmax(s, axis=-1).astype(vc.dtype)
        oc = jnp.einsum('bhqs,bshd->bqhd', p, vc).reshape(bsz, n_ctx, NA_W)
    return o, oc


def merge_branches(y_ssm, y_gqa, y_na, gate_logits, w_b_ssm, w_b_gqa, w_b_na, w_o):
    g = jax.nn.sigmoid(gate_logits.astype(jnp.float32)).astype(y_ssm.dtype)
    g = g.reshape(g.shape[:-1] + (N_BRANCHES, D_MODEL))
    m = (g[..., 0, :] * (y_ssm @ w_b_ssm)
         + g[..., 1, :] * (y_gqa @ w_b_gqa)
         + g[..., 2, :] * (y_na @ w_b_na))
    return m @ w_o


def swiglu(h, w_gate, w_up, w_down):
    return (jax.nn.silu(h @ w_gate) * (h @ w_up)) @ w_down


def moe_swiglu(h, router_w, w_gate, w_up, w_down):
    shp = h.shape
    t = h.reshape(-1, shp[-1])
    logits = (t @ router_w).astype(jnp.float32)
    top_v, top_i = lax.top_k(logits, TOP_K)
    top_w = jax.nn.softmax(top_v, axis=-1)
    gates = jnp.sum(jax.nn.one_hot(top_i, N_EXPERTS, dtype=jnp.float32) * top_w[..., None], axis=1)
    out = jnp.zeros_like(t)
    for e in range(N_EXPERTS):
        out = out + gates[:, e:e + 1].astype(t.dtype) * swiglu(t, w_gate[e], w_up[e], w_down[e])
    return out.reshape(shp)


def setup_inputs(seed: int = 0) -> dict:
    key = jax.random.key(seed)
    ks = iter(jax.random.split(key, 48))
    f32 = jnp.float32
    D = D_MODEL
    G, P, H = SSM_GROUPS, SSM_STATE, SSM_GROUP

    def nrm(shape, scale):
        return jax.random.normal(next(ks), shape, f32) * scale

    n_idx = jnp.arange(P, dtype=f32)
    return {
        "x": nrm((BATCH, SEQ, D), 1.0),
        "c": nrm((BATCH, D), 1.0),
        "ctx": nrm((BATCH, CTX_LEN, D), 1.0),
        "c_ctx": nrm((D,), 1.0),
        "w_mod": nrm((DEPTH, D, 6 * D), 0.5 * D ** -0.5),
        "b_mod": nrm((DEPTH, 6 * D), 0.01),
        "norm1_g": 1.0 + nrm((DEPTH, D), 0.02),
        "w_in": nrm((DEPTH, D, IN_COLS), D ** -0.5),
        "ssm_a_re": -0.5 + nrm((DEPTH, 2, G, P), 0.01),
        "ssm_a_im": math.pi * n_idx + nrm((DEPTH, 2, G, P), 0.01),
        "ssm_b_re": nrm((DEPTH, 2, G, P, H), (2 * H) ** -0.5),
        "ssm_b_im": nrm((DEPTH, 2, G, P, H), (2 * H) ** -0.5),
        "ssm_c_re": nrm((DEPTH, 2, G, H, P), P ** -0.5),
        "ssm_c_im": nrm((DEPTH, 2, G, H, P), P ** -0.5),
        "ssm_log_dt": jax.random.uniform(next(ks), (DEPTH, 2, G), f32, math.log(DT_MIN), math.log(DT_MAX)),
        "ssm_d": nrm((DEPTH, SSM_WIDTH), 1.0),
        "glu_w": nrm((DEPTH, SSM_WIDTH, SSM_WIDTH), SSM_WIDTH ** -0.5),
        "glu_b": nrm((DEPTH, SSM_WIDTH), 0.01),
        "q_norm_g": 1.0 + nrm((DEPTH, HEAD_DIM), 0.02),
        "k_norm_g": 1.0 + nrm((DEPTH, HEAD_DIM), 0.02),
        "na_rpb": nrm((DEPTH, NA_HEADS, 2 * NA_WIN_R - 1, 2 * NA_WIN_C - 1), 0.02),
        "w_branch_ssm": nrm((DEPTH, SSM_WIDTH, D), SSM_WIDTH ** -0.5),
        "w_branch_gqa": nrm((DEPTH, GQA_Q_W, D), GQA_Q_W ** -0.5),
        "w_branch_na": nrm((DEPTH, NA_W, D), NA_W ** -0.5),
        "w_out": nrm((DEPTH, D, D), D ** -0.5),
        "norm2_g": 1.0 + nrm((DEPTH, D), 0.02),
        "ffn_w_gate": nrm((N_DENSE, D, FFN_DIM), D ** -0.5),
        "ffn_w_up": nrm((N_DENSE, D, FFN_DIM), D ** -0.5),
        "ffn_w_down": nrm((N_DENSE, FFN_DIM, D), FFN_DIM ** -0.5),
        "router_w": nrm((N_MOE, D, N_EXPERTS), D ** -0.5),
        "moe_w_gate": nrm((N_MOE, N_EXPERTS, D, EXPERT_DIM), D ** -0.5),
        "moe_w_up": nrm((N_MOE, N_EXPERTS, D, EXPERT_DIM), D ** -0.5),
        "moe_w_down": nrm((N_MOE, N_EXPERTS, EXPERT_DIM, D), EXPERT_DIM ** -0.5),
        "final_norm_g": 1.0 + nrm((D,), 0.02),
    }


def reference(x, c, ctx, c_ctx, w_mod, b_mod, norm1_g, w_in, ssm_a_re, ssm_a_im, ssm_b_re, ssm_b_im,
              ssm_c_re, ssm_c_im, ssm_log_dt, ssm_d, glu_w, glu_b, q_norm_g, k_norm_g, na_rpb,
              w_branch_ssm, w_branch_gqa, w_branch_na, w_out, norm2_g, ffn_w_gate, ffn_w_up, ffn_w_down,
              router_w, moe_w_gate, moe_w_up, moe_w_down, final_norm_g):
    n_lat = x.shape[1]
    cos, sin = axial_rope(n_lat, x.dtype)
    xc = ctx
    for i in range(DEPTH):
        need_ctx = i < DEPTH - 1
        sh1, sc1, g1, sh2, sc2, g2 = [m[:, None, :] for m in adaln(c, w_mod[i], b_mod[i], 6)]
        mod_c = adaln(c_ctx, w_mod[i], b_mod[i], 6 if need_ctx else 2)

        h = modulate(rms_norm(x, norm1_g[i]), sh1, sc1)
        hc = modulate(rms_norm(xc, norm1_g[i]), mod_c[0], mod_c[1])
        z = h @ w_in[i]
        zc = hc @ (w_in[i] if need_ctx else w_in[i][:, :KV_COLS])
        u, ka, va, kn, vn, qa, qn, gl = jnp.split(z, IN_SPLITS, axis=-1)
        zc_parts = jnp.split(zc, IN_SPLITS if need_ctx else IN_SPLITS[:4], axis=-1)
        uc, kac, vac, knc, vnc = zc_parts[:5]
        qac, qnc, glc = zc_parts[5:] if need_ctx else (None, None, None)

        y_ssm, y_ssm_c = s5_bidirectional(u, uc, ssm_a_re[i], ssm_a_im[i], ssm_b_re[i], ssm_b_im[i],
                                          ssm_c_re[i], ssm_c_im[i], ssm_log_dt[i], ssm_d[i],
                                          glu_w[i], glu_b[i], need_ctx)
        y_gqa, y_gqa_c = gqa_branch(qa, ka, va, qac, kac, vac, q_norm_g[i], k_norm_g[i], cos, sin)
        y_na, y_na_c = na_branch(qn, kn, vn, qnc, knc, vnc, na_rpb[i])

        x = x + g1 * merge_branches(y_ssm, y_gqa, y_na, gl, w_branch_ssm[i], w_branch_gqa[i],
                                    w_branch_na[i], w_out[i])
        if need_ctx:
            xc = xc + mod_c[2] * merge_branches(y_ssm_c, y_gqa_c, y_na_c, glc, w_branch_ssm[i],
                                                w_branch_gqa[i], w_branch_na[i], w_out[i])

        j = i // 2
        if i % 2 == 0:
            ffn = functools.partial(swiglu, w_gate=ffn_w_gate[j], w_up=ffn_w_up[j], w_down=ffn_w_down[j])
        else:
            ffn = functools.partial(moe_swiglu, router_w=router_w[j], w_gate=moe_w_gate[j],
                                    w_up=moe_w_up[j], w_down=moe_w_down[j])
        x = x + g2 * ffn(modulate(rms_norm(x, norm2_g[i]), sh2, sc2))
        if need_ctx:
            xc = xc + mod_c[5] * ffn(modulate(rms_norm(xc, norm2_g[i]), mod_c[3], mod_c[4]))
    return rms_norm(x, final_norm_g)
```

```python
import contextlib
import math
import numpy as np
import ml_dtypes
import concourse.bass as bass
import concourse.mybir as mybir
from concourse.bass_utils import run_bass_kernel_spmd

F32 = mybir.dt.float32
BF16 = mybir.dt.bfloat16
U8 = mybir.dt.uint8
AF = mybir.ActivationFunctionType
ALU = mybir.AluOpType
AX = mybir.AxisListType

SAME_ENGINE_RAW_SYNC = True
SAME_ENGINE_FULL_SYNC = True
SSM_ENG = "dve"
N_DMA_SEMS = 8

D = 1024
NL = 4096
NC_ = 256
LT = NL + NC_
DEPTH = 2
IN_COLS = 5888
FFN_DIM = 2816
NE = 8
EXPERT_DIM = 3584
EPS = 1e-6
GRID_W = 64


class Buf:
    __slots__ = ("name", "t", "writer", "readers")

    def __init__(self, name, t=None):
        self.name = name
        self.t = t
        self.writer = None
        self.readers = []

    def __getitem__(self, k):
        return self.t[k]


class Op:
    __slots__ = ("stream", "fn", "waits", "signal", "sigval", "is_dma")

    def __init__(self, stream, fn, is_dma):
        self.stream = stream
        self.fn = fn
        self.waits = {}
        self.signal = False
        self.sigval = None
        self.is_dma = is_dma


class Prog:
    COMPUTE = ("pe", "act", "dve", "pool")
    QUEUES = {"q_sp": "sp", "q_act": "act", "q_pool": "pool"}

    def __init__(self, nc):
        self.nc = nc
        self.ops = {s: [] for s in ("pe", "act", "dve", "pool", "sp")}
        self.count = {s: 0 for s in self.COMPUTE}
        self.dcount = {q: 0 for q in self.QUEUES}
        self.stack = contextlib.ExitStack()
        self.sems = {s: self.stack.enter_context(nc.semaphore("s_" + s)) for s in self.COMPUTE}
        self.dsems = {q: [self.stack.enter_context(nc.semaphore("d_%s_%d" % (q, i))) for i in range(N_DMA_SEMS)]
                      for q in self.QUEUES}
        self.allops = {}
        self.order = {s: [] for s in self.COMPUTE}

    def _host(self, stream):
        return self.QUEUES.get(stream, stream)

    def _add(self, stream, fn, reads, writes):
        is_dma = stream in self.QUEUES
        host = self._host(stream)
        op = Op(stream, fn, is_dma)
        if is_dma:
            idx = self.dcount[stream]
            self.dcount[stream] += 1
            key = (stream, idx)
            if idx >= N_DMA_SEMS:
                self._dep(op, (stream, idx - N_DMA_SEMS))
        else:
            idx = self.count[stream]
            self.count[stream] += 1
            key = (stream, idx)
            self.order[stream].append(op)
        self.allops[key] = op
        for b in reads:
            if b.writer is not None:
                self._dep(op, b.writer, raw=True)
        for b in writes:
            if b.writer is not None:
                self._dep(op, b.writer)
            for r in b.readers:
                self._dep(op, r)
        for b in reads:
            b.readers.append(key)
        for b in writes:
            b.writer = key
            b.readers = []
        self.ops[host].append(op)
        return op

    def _dep(self, op, key, raw=False):
        pstream, pidx = key
        if pstream in self.QUEUES:
            wkey = ("d", pstream, pidx % N_DMA_SEMS)
            val = 16 * (pidx // N_DMA_SEMS + 1)
        else:
            if pstream == op.stream and not ((raw or SAME_ENGINE_FULL_SYNC) and SAME_ENGINE_RAW_SYNC and pstream != "pe"):
                return
            self.allops[key].signal = True
            wkey = ("c", pstream)
            val = pidx
        cur = op.waits.get(wkey)
        if cur is None or val > cur:
            op.waits[wkey] = val

    def op(self, stream, fn, reads=(), writes=()):
        return self._add(stream, fn, list(reads), list(writes))

    def dma(self, q, out, in_, reads=(), writes=(), **kw):
        return self._add(q, lambda e: e.dma_start(out=out, in_=in_, **kw), list(reads), list(writes))

    def barrier(self):
        lasts = []
        for s in self.COMPUTE:
            if self.count[s] > 0:
                lasts.append((s, self.count[s] - 1))
        for q in self.QUEUES:
            for i in range(max(0, self.dcount[q] - N_DMA_SEMS), self.dcount[q]):
                lasts.append((q, i))
        for host in ("pe", "act", "dve", "pool", "sp"):
            op = Op(host, (lambda e: e.nop()), False)
            if host in self.COMPUTE:
                idx = self.count[host]
                self.count[host] += 1
                self.allops[(host, idx)] = op
                self.order[host].append(op)
            for key in lasts:
                if key[0] == host:
                    continue
                self._dep(op, key)
            self.ops[host].append(op)

    def emit(self):
        nc = self.nc
        order = self.order
        for s in self.COMPUTE:
            c = 0
            for op in order[s]:
                if op.signal:
                    c += 1
                    op.sigval = c
        prog = self

        def run(host, e):
            waited = {}
            dq = {q: 0 for q in prog.QUEUES}
            for op in prog.ops[host]:
                for wkey, val in op.waits.items():
                    if wkey[0] == "d":
                        sem = prog.dsems[wkey[1]][wkey[2]]
                        v = val
                    else:
                        sem = prog.sems[wkey[1]]
                        v = order[wkey[1]][val].sigval
                    if waited.get(wkey, 0) >= v:
                        continue
                    waited[wkey] = v
                    e.wait_ge(sem, v)
                ins = op.fn(e)
                if op.is_dma:
                    i = dq[op.stream]
                    dq[op.stream] += 1
                    ins.then_inc(prog.dsems[op.stream][i % N_DMA_SEMS], 16)
                elif op.signal:
                    ins.then_inc(prog.sems[op.stream], 1)

        with nc.Block() as block:
            @block.tensor
            def _(e):
                run("pe", e)

            @block.scalar
            def _(e):
                run("act", e)

            @block.vector
            def _(e):
                run("dve", e)

            @block.gpsimd
            def _(e):
                run("pool", e)

            @block.sync
            def _(e):
                run("sp", e)
        self.stack.close()


class Ctx:
    def __init__(self, nc):
        self.nc = nc
        self.P = Prog(nc)
        self.cap = 200 * 1024
        self.big = nc.alloc_sbuf_tensor("big", [128, self.cap], U8)
        self.off = 0
        self.ps = [Buf("ps%d" % i, nc.alloc_psum_tensor("ps%d" % i, [128, 512], F32)) for i in range(8)]
        self.rr = {}

    def alloc(self, name, shape, dt):
        n = 1
        for s in shape:
            n *= s
        sz = n * (4 if dt == F32 else 2)
        sz = (sz + 63) // 64 * 64
        assert self.off + sz <= self.cap, "SBUF overflow %s: %d + %d" % (name, self.off, sz)
        ap = self.big[:, self.off:self.off + n * (4 if dt == F32 else 2)].bitcast(dt)
        self.off += sz
        if len(shape) == 2:
            ap = ap.rearrange("p (a b) -> p a b", a=shape[0])
        elif len(shape) == 3:
            ap = ap.rearrange("p (a b c) -> p a b c", a=shape[0], b=shape[1])
        return Buf(name, ap)

    def mark(self):
        return self.off

    def release(self, m):
        self.P.barrier()
        self.off = m

    def rot(self, key, lst):
        i = self.rr.get(key, 0)
        self.rr[key] = i + 1
        return lst[i % len(lst)]

    def mm(self, ps, out, lhsT, rhs, start, stop, reads, tp=None):
        kw = {}
        if tp is not None:
            kw["tile_position"] = tp
        return self.P.op("pe", lambda e: e.matmul(out, lhsT=lhsT, rhs=rhs, start=start, stop=stop, **kw),
                         reads=reads, writes=[ps])

    def act(self, ob, out, in_, func, reads, scale=1.0, bias=0.0):
        return self.P.op("act", lambda e: e.activation(out=out, in_=in_, func=func, bias=bias, scale=scale),
                         reads=reads, writes=[ob])

    def tt(self, eng, ob, out, in0, in1, op, reads):
        return self.P.op(eng, lambda e: e.tensor_tensor(out=out, in0=in0, in1=in1, op=op), reads=reads, writes=[ob])

    def ts(self, eng, ob, out, in0, s1, op0, reads, s2=None, op1=None):
        if op1 is None:
            return self.P.op(eng, lambda e: e.tensor_scalar(out=out, in0=in0, scalar1=s1, scalar2=None, op0=op0),
                             reads=reads, writes=[ob])
        return self.P.op(eng, lambda e: e.tensor_scalar(out=out, in0=in0, scalar1=s1, scalar2=s2, op0=op0, op1=op1),
                         reads=reads, writes=[ob])

    def stt(self, ob, out, in0, scalar, in1, op0, op1, reads):
        return self.P.op("dve", lambda e: e.scalar_tensor_tensor(out=out, in0=in0, scalar=scalar, in1=in1, op0=op0, op1=op1),
                         reads=reads, writes=[ob])

    def copy(self, eng, ob, out, in_, reads):
        if eng == "act":
            return self.P.op("act", lambda e: e.copy(out=out, in_=in_), reads=reads, writes=[ob])
        return self.P.op(eng, lambda e: e.tensor_copy(out=out, in_=in_), reads=reads, writes=[ob])

    def recip(self, ob, out, in_, reads):
        return self.P.op("dve", lambda e: e.reciprocal(out=out, in_=in_), reads=reads, writes=[ob])

    def memset(self, eng, ob, out, val):
        return self.P.op(eng, lambda e: e.memset(out, val), reads=[], writes=[ob])


def dump(C, T, name, ap, buf, dt=None):
    if name not in T.get("_debug", ()):
        return
    d = C.nc.dram_tensor("dbg_" + name, list(ap.shape), dt or F32, kind="ExternalOutput").ap()
    C.P.dma("q_sp", d, ap, reads=[buf])


TOK_TILES = [(i * 512, 512, 0) for i in range(8)] + [(NL, 256, 1)]


def phase_setup(C, T):
    P = C.P
    m = C.mark()
    xin = [C.alloc("xin%d" % i, (1024,), F32) for i in range(2)]
    xst = [C.alloc("xst%d" % i, (8, 128), F32) for i in range(2)]
    XTv = T["XT"].rearrange("(k p) t -> p k t", p=128)
    for tt in range(LT // 128):
        t0 = tt * 128
        xi = C.rot("xin", xin)
        st = C.rot("xst", xst)
        src = T["x"][t0:t0 + 128, :] if t0 < NL else T["ctx"][t0 - NL:t0 - NL + 128, :]
        P.dma("q_sp", xi[:, :], src, writes=[xi])
        for half in range(2):
            ps = C.rot("ps_setup", C.ps[0:4])
            for kk in range(4):
                k = half * 4 + kk
                C.mm(ps, ps[:, kk * 128:(kk + 1) * 128], xi[:, k * 128:(k + 1) * 128], T["identF"][:, :], True, True,
                     [xi, T["identF_b"]])
            eng = "act" if half == 0 else "dve"
            C.copy(eng, st, st[:, half * 4:(half + 1) * 4, :], ps[:, :].rearrange("p (a b) -> p a b", a=4), [ps])
        P.dma("q_act", XTv[:, :, t0:t0 + 128], st[:, :, :], reads=[st], writes=[T["XT_b"]])
    C.release(m)


def load_vec_fm(C, dst_buf, dst_ap, src_ap, q="q_sp"):
    C.P.dma(q, dst_ap, src_ap.rearrange("(j p) -> p j", p=128), writes=[dst_buf], allow_slow_non_contiguous=True)


def phase_mod(C, T, l):
    P = C.P
    MOD, A1, A2 = T["MOD"], T["A1"], T["A2"]
    m = C.mark()
    cv = C.alloc("cv", (8, 2), F32)
    sc = C.alloc("sc", (8, 2), F32)
    bm = C.alloc("bm", (48,), F32)
    gn = C.alloc("gn", (2, 8), F32)
    wm = [C.alloc("wm%d" % i, (8, 512), F32) for i in range(2)]
    P.dma("q_sp", cv[:, :, 0], T["c"].rearrange("(j p) -> p j", p=128), writes=[cv], allow_slow_non_contiguous=True)
    P.dma("q_sp", cv[:, :, 1], T["c_ctx"].rearrange("(j p) -> p j", p=128), writes=[cv], allow_slow_non_contiguous=True)
    load_vec_fm(C, bm, bm[:, :], T["b_mod"][l])
    load_vec_fm(C, gn, gn[:, 0, :], T["norm1_g"][l])
    load_vec_fm(C, gn, gn[:, 1, :], T["norm2_g"][l])
    C.act(sc, sc[:, :, :], cv[:, :, :], AF.Silu, [cv])
    wv = T["w_mod"][l].rearrange("(k p) c -> p k c", p=128)
    for grp in range(12):
        w = C.rot("wm", wm)
        P.dma("q_sp" if grp % 2 == 0 else "q_act", w[:, :, :], wv[:, :, grp * 512:(grp + 1) * 512], writes=[w])
        ps = C.rot("ps_mod", C.ps[0:2])
        for jj in range(4):
            for k in range(8):
                C.mm(ps, ps[:, jj * 2:jj * 2 + 2], w[:, k, jj * 128:(jj + 1) * 128], sc[:, k, :], k == 0, k == 7, [w, sc])
        for jj in range(4):
            j = grp * 4 + jj
            C.ts("dve", MOD, MOD[:, j, :], ps[:, jj * 2:jj * 2 + 2], bm[:, j:j + 1], ALU.add, [ps, bm])
    for who in range(2):
        C.stt(A1, A1[:, :, who], MOD[:, 8:16, who], 1.0, gn[:, 0, :], ALU.add, ALU.mult, [MOD, gn])
        C.stt(A2, A2[:, :, who], MOD[:, 32:40, who], 1.0, gn[:, 1, :], ALU.add, ALU.mult, [MOD, gn])
    C.release(m)


def norm_modulate(C, T, xt, n, who, A, shift_chunk, outs, tmp, sq, rstd, ps_ss):
    MOD = T["MOD"]
    C.act(sq, sq[:, :, 0:n], xt[:, :, 0:n], AF.Square, [xt])
    for k in range(8):
        C.mm(ps_ss, ps_ss[:, 0:n], T["onesF"][:, :], sq[:, k, 0:n], k == 0, k == 7, [sq, T["onesF_b"]])
    C.act(rstd, rstd[:, 0:n], ps_ss[:, 0:n], AF.Sqrt, [ps_ss], scale=1.0 / D, bias=T["eps"][:, 0:1])
    C.recip(rstd, rstd[:, 0:n], rstd[:, 0:n], [rstd])
    for k in range(8):
        C.tt("dve", tmp, tmp[:, k, 0:n], xt[:, k, 0:n], rstd[:, 0:n], ALU.mult, [xt, rstd])
        for (ob, fn) in outs:
            C.act(ob, fn(k), tmp[:, k, 0:n], AF.Identity, [tmp, A, MOD],
                  scale=A[:, k, who:who + 1], bias=MOD[:, shift_chunk * 8 + k, who:who + 1])


def phase_inproj(C, T, l):
    P = C.P
    m = C.mark()
    HT = C.alloc("HT", (8, LT), BF16)
    XTv = T["XT"].rearrange("(k p) t -> p k t", p=128)
    m1 = C.mark()
    xt = [C.alloc("xt%d" % i, (8, 512), F32) for i in range(2)]
    tmp = C.alloc("tmp", (8, 512), F32)
    sq = C.alloc("sq", (8, 512), F32)
    rstd = C.alloc("rstd", (512,), F32)
    for (t0, n, who) in TOK_TILES:
        x_ = C.rot("xt", xt)
        P.dma("q_sp", x_[:, :, 0:n], XTv[:, :, t0:t0 + n], reads=[T["XT_b"]], writes=[x_])
        norm_modulate(C, T, x_, n, who, T["A1"], 0, [(HT, lambda k, t0=t0, n=n: HT[:, k, t0:t0 + n])],
                      tmp, sq, rstd, C.ps[7])
    C.release(m1)
    gq = C.alloc("gq", (1,), F32)
    gk = C.alloc("gk", (1,), F32)
    for h in range(2):
        P.dma("q_sp", gq[h * 64:(h + 1) * 64, :], T["q_norm_g"][l].rearrange("(p o) -> p o", o=1), writes=[gq],
              allow_slow_non_contiguous=True)
        P.dma("q_sp", gk[h * 64:(h + 1) * 64, :], T["k_norm_g"][l].rearrange("(p o) -> p o", o=1), writes=[gk],
              allow_slow_non_contiguous=True)
    cosT = C.alloc("cosT", (LT,), BF16)
    sinT = C.alloc("sinT", (LT,), BF16)
    P.dma("q_sp", cosT[:, :], T["cos"], writes=[cosT])
    P.dma("q_sp", sinT[:, :], T["sin"], writes=[sinT])
    wt = [C.alloc("wt%d" % i, (8, 512), BF16) for i in range(2)]
    stg = [C.alloc("stg%d" % i, (512,), BF16) for i in range(4)]
    sqh = C.alloc("sqh", (512,), F32)
    rs = C.alloc("rs", (512,), F32)
    qn = C.alloc("qn", (512,), BF16)
    t1 = C.alloc("t1", (512,), F32)
    wv_ = T["w_in"][l].rearrange("(k p) c -> p k c", p=128)
    dests = {}
    for cb in range(46):
        if cb < 4:
            dests[cb] = ("copy", T["UT"], T["UT_b"], cb)
        elif cb == 4:
            dests[cb] = ("rope", T["KGT"], T["KGT_b"], 0, gk)
        elif cb == 5:
            dests[cb] = None
        elif cb < 10:
            dests[cb] = ("copy", T["KNT"], T["KNT_b"], cb - 6)
        elif cb < 14:
            dests[cb] = None
        elif cb < 18:
            dests[cb] = ("rope", T["QGT"], T["QGT_b"], cb - 14, gq)
        elif cb < 22:
            dests[cb] = ("copy", T["QNT"], T["QNT_b"], cb - 18)
        else:
            dests[cb] = ("sig", T["GT"], T["GT_b"], cb - 22)
    groups = [(g * 512, 512) for g in range(11)] + [(5632, 256)]
    for (c0, ncols) in groups:
        w = C.rot("wt", wt)
        P.dma("q_pool", w[:, :, 0:ncols], wv_[:, :, c0:c0 + ncols], writes=[w])
        for bl in range(ncols // 128):
            cb = c0 // 128 + bl
            dd = dests[cb]
            if dd is None:
                continue
            for (t0, n, who) in TOK_TILES:
                ps = C.rot("ps_in", C.ps[0:3])
                for k in range(8):
                    C.mm(ps, ps[:, 0:n], w[:, k, bl * 128:(bl + 1) * 128], HT[:, k, t0:t0 + n], k == 0, k == 7, [w, HT])
                st = C.rot("stg", stg)
                kind, dt_, db, blk = dd[0], dd[1], dd[2], dd[3]
                if kind == "copy":
                    C.copy("act", st, st[:, 0:n], ps[:, 0:n], [ps])
                elif kind == "sig":
                    C.act(st, st[:, 0:n], ps[:, 0:n], AF.Sigmoid, [ps])
                else:
                    gvec = dd[4]
                    pss, psr = C.ps[3], C.ps[4]
                    C.act(sqh, sqh[:, 0:n], ps[:, 0:n], AF.Square, [ps])
                    C.mm(pss, pss[:, 0:n], T["blk64F"][:, :], sqh[:, 0:n], True, True, [sqh, T["blk64F_b"]])
                    C.act(rs, rs[:, 0:n], pss[:, 0:n], AF.Sqrt, [pss], scale=1.0 / 64, bias=T["eps"][:, 0:1])
                    C.recip(rs, rs[:, 0:n], rs[:, 0:n], [rs])
                    C.stt(qn, qn[:, 0:n], ps[:, 0:n], gvec[:, 0:1], rs[:, 0:n], ALU.mult, ALU.mult, [ps, gvec, rs])
                    C.mm(psr, psr[:, 0:n], T["rotM"][:, :], qn[:, 0:n], True, True, [qn, T["rotM_b"]])
                    C.tt("dve", t1, t1[:, 0:n], qn[:, 0:n], cosT[:, t0:t0 + n], ALU.mult, [qn, cosT])
                    C.tt("dve", rs, rs[:, 0:n], psr[:, 0:n], sinT[:, t0:t0 + n], ALU.mult, [psr, sinT])
                    C.tt("dve", st, st[:, 0:n], t1[:, 0:n], rs[:, 0:n], ALU.add, [t1, rs])
                P.dma("q_sp", dt_[blk * 128:(blk + 1) * 128, t0:t0 + n], st[:, 0:n], reads=[st], writes=[db])
    wvv = C.alloc("wvv", (8, 640), BF16)
    P.dma("q_pool", wvv[:, :, 0:128], wv_[:, :, 640:768], writes=[wvv])
    P.dma("q_pool", wvv[:, :, 128:640], wv_[:, :, 1280:1792], writes=[wvv])
    vst = [C.alloc("vst%d" % i, (640,), BF16) for i in range(2)]
    for tt in range(LT // 128):
        t0 = tt * 128
        pa = C.rot("ps_va", C.ps[0:2])
        pb = C.rot("ps_vb", C.ps[2:4])
        for k in range(8):
            C.mm(pa, pa[:, 0:128], HT[:, k, t0:t0 + 128], wvv[:, k, 0:128], k == 0, k == 7, [HT, wvv])
        for k in range(8):
            C.mm(pb, pb[:, 0:512], HT[:, k, t0:t0 + 128], wvv[:, k, 128:640], k == 0, k == 7, [HT, wvv])
        vs = C.rot("vst", vst)
        C.copy("act", vs, vs[:, 0:128], pa[:, 0:128], [pa])
        C.copy("dve", vs, vs[:, 128:640], pb[:, 0:512], [pb])
        P.dma("q_sp", T["VG"][t0:t0 + 128, :], vs[:, 0:128], reads=[vs], writes=[T["VG_b"]])
        P.dma("q_act", T["VN"][t0:t0 + 128, :], vs[:, 128:640], reads=[vs], writes=[T["VN_b"]])
    C.release(m)


def _attn_finish(C, T, OD, n, rcb, bcs, stg, psB, dst_ap, dst_b):
    C.recip(rcb, rcb[64:65, 0:n], OD[64:65, 0:n], [OD])
    C.mm(psB, psB[0:64, 0:n], T["onesF"][64:65, 0:64], rcb[64:65, 0:n], True, True, [T["onesF_b"], rcb])
    C.copy("act", bcs, bcs[0:64, 0:n], psB[0:64, 0:n], [psB])
    st = C.rot("afst", stg)
    C.tt("dve", st, st[0:64, 0:n], OD[0:64, 0:n], bcs[0:64, 0:n], ALU.mult, [OD, bcs])
    C.P.dma("q_act", dst_ap, st[0:64, 0:n], reads=[st], writes=[dst_b])


def phase_gqa(C, T, l, need_ctx):
    P = C.P
    m = C.mark()
    LA = 1
    KK = [C.alloc("KK%d" % kv, (LT,), BF16) for kv in range(2)]
    for kv in range(2):
        for half in range(2):
            P.dma("q_sp" if half == 0 else "q_act", KK[kv][half * 64:(half + 1) * 64, :],
                  T["KGT"][kv * 64:(kv + 1) * 64, :], reads=[T["KGT_b"]], writes=[KK[kv]])
    V = C.alloc("Vg", (34, 2, 65), BF16)
    for kv in range(2):
        P.dma("q_sp", V[:, :, kv, 0:64], T["VG"].rearrange("(t p) c -> p t c", p=128)[:, :, kv * 64:(kv + 1) * 64],
              reads=[T["VG_b"]], writes=[V])
    C.memset("dve", V, V[:, :, :, 64], 1.0)
    qb = [C.alloc("qb%d" % i, (512,), BF16) for i in range(2)]
    pts = [C.alloc("pt%d" % i, (512,), BF16) for i in range(4)]
    rcb = C.alloc("rcb", (512,), F32)
    bcs = C.alloc("bcs", (512,), F32)
    stg = [C.alloc("gst%d" % i, (512,), BF16) for i in range(2)]
    tiles = TOK_TILES if need_ctx else TOK_TILES[:8]
    items = []
    for (t0, n, who) in tiles:
        kts = list(range(34)) if who == 0 else [32, 33]
        for j in range(4):
            for ki, kt in enumerate(kts):
                items.append((t0, n, j, ki, kt, len(kts)))
    state = {}
    for idx in range(len(items) + LA):
        if idx < len(items):
            (t0, n, j, ki, kt, nk) = items[idx]
            if ki == 0:
                q = C.rot("qb", qb)
                P.dma("q_sp", q[:, 0:n], T["QGT"][j * 128:(j + 1) * 128, t0:t0 + n], reads=[T["QGT_b"]], writes=[q])
                state["q"] = q
            q = state["q"]
            kv = (2 * j) // 4
            Ss = [C.rot("psS", C.ps[0:4]) for p in range(2)]
            for p in range(2):
                C.mm(Ss[p], Ss[p][:, 0:n], KK[kv][p * 64:(p + 1) * 64, kt * 128:(kt + 1) * 128], q[p * 64:(p + 1) * 64, 0:n],
                     True, True, [KK[kv], q])
            ptp = []
            for p in range(2):
                pt = C.rot("pts", pts)
                C.act(pt, pt[:, 0:n], Ss[p][:, 0:n], AF.Exp, [Ss[p]], scale=0.125)
                ptp.append(pt)
            state[idx] = ptp
        if idx >= LA:
            (t0, n, j, ki, kt, nk) = items[idx - LA]
            ptp = state.pop(idx - LA)
            kv = (2 * j) // 4
            if ki == 0:
                state["OD"] = [C.rot("psO", C.ps[4:8]) for p in range(2)]
            ODs = state["OD"]
            for p in range(2):
                C.mm(ODs[p], ODs[p][0:65, 0:n], V[:, kt, kv, :], ptp[p][:, 0:n], ki == 0, ki == nk - 1, [V, ptp[p]])
            if ki == nk - 1:
                for p in range(2):
                    h = 2 * j + p
                    _attn_finish(C, T, ODs[p], n, rcb, bcs, stg, C.rot("psS", C.ps[0:4]),
                                 T["YGT"][h * 64:(h + 1) * 64, t0:t0 + n], T["YGT_b"])
    C.release(m)


def phase_na(C, T, l, need_ctx):
    P = C.P
    m = C.mark()
    LA = 1
    BT = C.alloc("BT", (8, 14, 64), BF16)
    m1 = C.mark()
    bst = C.alloc("bst", (8, 14, 64), F32)
    msk = C.alloc("msk", (8, 14, 64), BF16)
    for il in range(2):
        for h in range(8):
            P.dma("q_sp" if h % 2 == 0 else "q_act", bst[il * 64:(il + 1) * 64, h, :, :],
                  T["rpbT"][l, h, il:il + 14, :, :].rearrange("r k q -> k r q"), writes=[bst])
    P.dma("q_act", msk[:, :, :, :], T["namask"], writes=[msk])
    C.stt(BT, BT[:, :, :, :].rearrange("p a b c -> p (a b c)"), bst[:, :, :, :].rearrange("p a b c -> p (a b c)"), 8.0,
          msk[:, :, :, :].rearrange("p a b c -> p (a b c)"), ALU.mult, ALU.add, [bst, msk])
    C.release(m1)
    KT = C.alloc("KTn", (4, LT), BF16)
    QT = C.alloc("QTn", (4, LT), BF16)
    V1 = C.alloc("V1", (34, 8, 65), BF16)
    V2 = C.alloc("V2", (33, 8, 65), BF16)
    P.dma("q_sp", KT[:, :, :], T["KNT"].rearrange("(j p) t -> p j t", p=128), reads=[T["KNT_b"]], writes=[KT])
    P.dma("q_act", QT[:, :, :], T["QNT"].rearrange("(j p) t -> p j t", p=128), reads=[T["QNT_b"]], writes=[QT])
    for h in range(8):
        P.dma("q_sp", V1[:, :, h, 0:64], T["VN"].rearrange("(t p) c -> p t c", p=128)[:, :, h * 64:(h + 1) * 64],
              reads=[T["VN_b"]], writes=[V1])
        P.dma("q_act", V2[:, :, h, 0:64], T["VN"][64:64 + 33 * 128, :].rearrange("(t p) c -> p t c", p=128)[:, :, h * 64:(h + 1) * 64],
              reads=[T["VN_b"]], writes=[V2])
    for h in range(8):
        C.memset("dve", V1, V1[:, :, h, 64], 1.0)
        C.memset("pool", V2, V2[:, :, h, 64], 1.0)
    identB = C.alloc("identB", (128,), BF16)
    C.copy("dve", identB, identB[:, :], T["identF"][:, :], [T["identF_b"]])
    pts = [C.alloc("npt%d" % i, (512,), BF16) for i in range(4)]
    rcb = C.alloc("nrcb", (512,), F32)
    bcs = C.alloc("nbcs", (512,), F32)
    stg = [C.alloc("nst%d" % i, (512,), BF16) for i in range(2)]
    items = []
    for j in range(4):
        for rb in range(8):
            for rl in range(8):
                items.append(("lat", j, rb, rl))
        if need_ctx:
            items.append(("ctx", j, 0, 0))
    state = {}
    for idx in range(len(items) + LA):
        if idx < len(items):
            (kind, j, rb, rl) = items[idx]
            Ss = [C.rot("psS", C.ps[0:4]) for p in range(2)]
            ptp = [C.rot("npts", pts) for p in range(2)]
            hs = [slice(p * 64, (p + 1) * 64) for p in range(2)]
            if kind == "lat":
                r = rb * 8 + rl
                rs_ = min(max(r - 4, 0), 56)
                kb = rs_ * 64
                for c in range(4):
                    pi = 2 * c + 7 - (r - rs_)
                    for p in range(2):
                        C.mm(Ss[p], Ss[p][:, c * 64:(c + 1) * 64], KT[hs[p], j, kb + c * 128:kb + (c + 1) * 128],
                             QT[hs[p], j, r * 64:(r + 1) * 64], True, False, [KT, QT])
                    for p in range(2):
                        C.mm(Ss[p], Ss[p][:, c * 64:(c + 1) * 64], identB[:, :], BT[:, 2 * j + p, pi, :], False, True, [identB, BT])
                for cc in range(2):
                    for p in range(2):
                        C.mm(Ss[p], Ss[p][:, 256 + cc * 64:256 + (cc + 1) * 64], KT[hs[p], j, NL + cc * 128:NL + (cc + 1) * 128],
                             QT[hs[p], j, r * 64:(r + 1) * 64], True, True, [KT, QT])
                for p in range(2):
                    C.act(ptp[p], ptp[p][:, 0:384], Ss[p][:, 0:384], AF.Exp, [Ss[p]], scale=0.125)
            else:
                for cc in range(2):
                    for p in range(2):
                        C.mm(Ss[p], Ss[p][:, cc * 256:(cc + 1) * 256], KT[hs[p], j, NL + cc * 128:NL + (cc + 1) * 128],
                             QT[hs[p], j, NL:NL + 256], True, True, [KT, QT])
                for p in range(2):
                    C.act(ptp[p], ptp[p][:, 0:512], Ss[p][:, 0:512], AF.Exp, [Ss[p]], scale=0.125)
            state[idx] = ptp
        if idx >= LA:
            (kind, j, rb, rl) = items[idx - LA]
            ptp = state.pop(idx - LA)
            if rl == 0:
                state["OD"] = [C.rot("psO", C.ps[4:8]) for p in range(2)]
            ODs = state["OD"]
            if kind == "lat":
                r = rb * 8 + rl
                rs_ = min(max(r - 4, 0), 56)
                for c in range(6):
                    for p in range(2):
                        h = 2 * j + p
                        if c < 4:
                            vt = V1[:, rs_ // 2 + c, h, :] if rs_ % 2 == 0 else V2[:, (rs_ - 1) // 2 + c, h, :]
                        else:
                            vt = V1[:, 32 + (c - 4), h, :]
                        C.mm(ODs[p], ODs[p][0:65, rl * 64:(rl + 1) * 64], vt, ptp[p][:, c * 64:(c + 1) * 64], c == 0, c == 5,
                             [V1, V2, ptp[p]])
                if rl == 7:
                    for p in range(2):
                        h = 2 * j + p
                        _attn_finish(C, T, ODs[p], 512, rcb, bcs, stg, C.rot("psS", C.ps[0:4]),
                                     T["YNT"][h * 64:(h + 1) * 64, rb * 512:(rb + 1) * 512], T["YNT_b"])
            else:
                for cc in range(2):
                    for p in range(2):
                        h = 2 * j + p
                        C.mm(ODs[p], ODs[p][0:65, 0:256], V1[:, 32 + cc, h, :], ptp[p][:, cc * 256:(cc + 1) * 256], cc == 0, cc == 1,
                             [V1, ptp[p]])
                for p in range(2):
                    h = 2 * j + p
                    _attn_finish(C, T, ODs[p], 256, rcb, bcs, stg, C.rot("psS", C.ps[0:4]),
                                 T["YNT"][h * 64:(h + 1) * 64, NL:NL + 256], T["YNT_b"])
    C.release(m)


def phase_ssm(C, T, l, need_ctx):
    P = C.P
    m = C.mark()
    TT = 128
    NCH = LT // TT
    uT = C.alloc("uT", (4, LT), BF16)
    P.dma("q_sp", uT[:, :, :], T["UT"].rearrange("(k p) t -> p k t", p=128), reads=[T["UT_b"]], writes=[uT])
    Yacc = C.alloc("Yacc", (4, LT), BF16)
    dvec = C.alloc("dvec", (4,), F32)
    load_vec_fm(C, dvec, dvec[:, :], T["ssm_d"][l])
    halfpi = C.alloc("halfpi", (1,), F32)
    C.memset("dve", halfpi, halfpi[:, :], math.pi / 2)
    rm = C.alloc("rm", (16,), F32)
    P.dma("q_sp", rm[:, :], T["rowmask"], writes=[rm])
    identF = T["identF"]
    for d in (1, 0):
        first = (d == 1)
        m2 = C.mark()
        sm = lambda nm, n=16: C.alloc(nm, (n,), F32)
        ldt, are, aim, dt, rmag, th, s_, c_, t_, s2 = [sm(x) for x in ("ldt", "are", "aim", "dt", "rmag", "th", "s_", "c_", "t_", "s2")]
        Wre = C.alloc("Wre", (8, 16), F32)
        Wim = C.alloc("Wim", (8, 16), F32)
        abr, abi, nr, den, kr, ki, nki, ta, tb = [sm(x) for x in ("abr", "abi", "nr", "den", "kr", "ki", "nki", "ta", "tb")]
        for gl in range(2):
            sl = slice(gl * 64, (gl + 1) * 64)
            P.dma("q_sp", ldt[sl, :], T["ssm_log_dt"][l, d].rearrange("(cg gl) -> gl cg", gl=2)[gl:gl + 1, :].partition_broadcast(64),
                  writes=[ldt], allow_slow_non_contiguous=True)
            P.dma("q_sp", are[sl, :], T["ssm_a_re"][l, d].rearrange("(cg gl) p -> gl p cg", gl=2)[gl], writes=[are],
                  allow_slow_non_contiguous=True)
            P.dma("q_act", aim[sl, :], T["ssm_a_im"][l, d].rearrange("(cg gl) p -> gl p cg", gl=2)[gl], writes=[aim],
                  allow_slow_non_contiguous=True)
        C.act(dt, dt[:, :], ldt[:, :], AF.Exp, [ldt])
        C.tt("dve", ta, ta[:, :], are[:, :], dt[:, :], ALU.mult, [are, dt])
        C.act(rmag, rmag[:, :], ta[:, :], AF.Exp, [ta])
        C.tt("dve", th, th[:, :], aim[:, :], dt[:, :], ALU.mult, [aim, dt])
        C.act(s_, s_[:, :], th[:, :], AF.Sin, [th], scale=1.0 / 16)
        C.act(c_, c_[:, :], th[:, :], AF.Sin, [th, halfpi], scale=1.0 / 16, bias=halfpi[:, 0:1])
        for _ in range(4):
            C.tt("dve", t_, t_[:, :], s_[:, :], c_[:, :], ALU.mult, [s_, c_])
            C.tt("dve", s2, s2[:, :], s_[:, :], s_[:, :], ALU.mult, [s_])
            C.ts("dve", s_, s_[:, :], t_[:, :], 2.0, ALU.mult, [t_])
            C.ts("dve", c_, c_[:, :], s2[:, :], -2.0, ALU.mult, [s2], s2=1.0, op1=ALU.add)
        C.copy("dve", Wre, Wre[:, 0, :], c_[:, :], [c_])
        C.copy("dve", Wim, Wim[:, 0, :], s_[:, :], [s_])
        for k in range(7):
            C.tt("dve", ta, ta[:, :], Wre[:, k, :], Wre[:, k, :], ALU.mult, [Wre])
            C.tt("dve", tb, tb[:, :], Wim[:, k, :], Wim[:, k, :], ALU.mult, [Wim])
            C.tt("dve", Wre, Wre[:, k + 1, :], ta[:, :], tb[:, :], ALU.subtract, [ta, tb])
            C.tt("dve", ta, ta[:, :], Wre[:, k, :], Wim[:, k, :], ALU.mult, [Wre, Wim])
            C.ts("dve", Wim, Wim[:, k + 1, :], ta[:, :], 2.0, ALU.mult, [ta])
        C.tt("dve", abr, abr[:, :], rmag[:, :], c_[:, :], ALU.mult, [rmag, c_])
        C.tt("dve", abi, abi[:, :], rmag[:, :], s_[:, :], ALU.mult, [rmag, s_])
        C.ts("dve", nr, nr[:, :], abr[:, :], -1.0, ALU.add, [abr])
        C.tt("dve", ta, ta[:, :], are[:, :], are[:, :], ALU.mult, [are])
        C.tt("dve", tb, tb[:, :], aim[:, :], aim[:, :], ALU.mult, [aim])
        C.tt("dve", den, den[:, :], ta[:, :], tb[:, :], ALU.add, [ta, tb])
        C.recip(den, den[:, :], den[:, :], [den])
        C.tt("dve", ta, ta[:, :], nr[:, :], are[:, :], ALU.mult, [nr, are])
        C.tt("dve", tb, tb[:, :], abi[:, :], aim[:, :], ALU.mult, [abi, aim])
        C.tt("dve", ta, ta[:, :], ta[:, :], tb[:, :], ALU.add, [ta, tb])
        C.tt("dve", kr, kr[:, :], ta[:, :], den[:, :], ALU.mult, [ta, den])
        C.tt("dve", ta, ta[:, :], abi[:, :], are[:, :], ALU.mult, [abi, are])
        C.tt("dve", tb, tb[:, :], nr[:, :], aim[:, :], ALU.mult, [nr, aim])
        C.tt("dve", ta, ta[:, :], ta[:, :], tb[:, :], ALU.subtract, [ta, tb])
        C.tt("dve", ki, ki[:, :], ta[:, :], den[:, :], ALU.mult, [ta, den])
        C.ts("dve", nki, nki[:, :], ki[:, :], -1.0, ALU.mult, [ki])
        WB = C.alloc("WB", (16, 2, 128), BF16)
        WC = C.alloc("WC", (16, 2, 128), BF16)
        if d == 0:
            for nm, bf in (("rmag", rmag), ("th", th), ("kr", kr), ("ki", ki), ("c_", c_), ("s_", s_)):
                dump(C, T, nm, bf[:, :], bf)
            dump(C, T, "Wre", Wre[:, :, :], Wre)
        m3 = C.mark()
        bre = C.alloc("bre", (16, 16), F32)
        bim = C.alloc("bim", (16, 16), F32)
        for gl in range(2):
            sl = slice(gl * 64, (gl + 1) * 64)
            P.dma("q_sp", bre[sl, :, :], T["ssm_b_re"][l, d].rearrange("(cg gl) p h -> gl p cg h", gl=2)[gl], writes=[bre])
            P.dma("q_act", bim[sl, :, :], T["ssm_b_im"][l, d].rearrange("(cg gl) p h -> gl p cg h", gl=2)[gl], writes=[bim])
        Bxr = C.alloc("Bxr", (16, 128), F32)
        Bxi = C.alloc("Bxi", (16, 128), F32)
        C.memset("pool", Bxr, Bxr[:, :, :], 0.0)
        C.memset("pool", Bxi, Bxi[:, :, :], 0.0)
        tq = C.alloc("tq", (16,), F32)
        for cg in range(16):
            for gl in range(2):
                sl = slice(gl * 64, (gl + 1) * 64)
                off = ((2 * cg + gl) % 8) * 16
                C.ts("dve", tq, tq[sl, :], bre[sl, cg, :], kr[sl, cg:cg + 1], ALU.mult, [bre, kr])
                C.stt(Bxr, Bxr[sl, cg, off:off + 16], bim[sl, cg, :], nki[sl, cg:cg + 1], tq[sl, :], ALU.mult, ALU.add, [bim, nki, tq])
                C.ts("dve", tq, tq[sl, :], bim[sl, cg, :], kr[sl, cg:cg + 1], ALU.mult, [bim, kr])
                C.stt(Bxi, Bxi[sl, cg, off:off + 16], bre[sl, cg, :], ki[sl, cg:cg + 1], tq[sl, :], ALU.mult, ALU.add, [bre, ki, tq])
        for cg in range(16):
            ps = C.rot("ps_w", C.ps[0:4])
            C.mm(ps, ps[:, 0:128], Bxr[:, cg, :], identF[:, :], True, True, [Bxr, identF])
            C.mm(ps, ps[:, 128:256], Bxi[:, cg, :], identF[:, :], True, True, [Bxi, identF])
            C.copy("act", WB, WB[:, cg, :, :], ps[:, 0:256].rearrange("p (a b) -> p a b", a=2), [ps])
        Cn = [C.alloc("Cnre", (4, 64), F32), C.alloc("Cnim", (4, 64), F32)]
        P.dma("q_sp", Cn[0][:, :, :], T["ssm_c_re"][l, d].rearrange("(a g) h p -> (g h) a p", a=4), writes=[Cn[0]])
        P.dma("q_act", Cn[1][:, :, :], T["ssm_c_im"][l, d].rearrange("(a g) h p -> (g h) a p", a=4), writes=[Cn[1]])
        Xs = [C.alloc("Xs%d" % i, (128,), F32) for i in range(4)]
        for cg in range(16):
            ps = C.rot("ps_w", C.ps[0:4])
            for x in range(2):
                Xt = C.rot("Xs", Xs)
                for gl in range(2):
                    k = (2 * cg + gl) % 8 + (8 if x == 1 else 0)
                    C.ts("dve", Xt, Xt[:, gl * 64:(gl + 1) * 64], Cn[x][:, cg // 4, :], rm[:, k:k + 1], ALU.mult, [Cn[x], rm])
                C.mm(ps, ps[:, x * 128:(x + 1) * 128], Xt[:, :], identF[:, :], True, True, [Xt, identF])
            C.copy("act", WC, WC[:, cg, :, :], ps[:, 0:256].rearrange("p (a b) -> p a b", a=2), [ps])
        if d == 0:
            dump(C, T, "WB", WB[:, :, :, :].rearrange("p a b c -> p (a b c)"), WB, BF16)
            dump(C, T, "WC", WC[:, :, :, :].rearrange("p a b c -> p (a b c)"), WC, BF16)
        C.release(m3)
        cosT = C.alloc("scos", (16, TT), F32)
        sinT = C.alloc("ssin", (16, TT), F32)
        m4 = C.mark()
        tA = C.alloc("tA", (16, 64), F32)
        tB = C.alloc("tB", (16, 64), F32)
        C.memset("dve", cosT, cosT[:, :, 0:1], 1.0)
        C.memset("dve", sinT, sinT[:, :, 0:1], 0.0)
        for k in range(7):
            st = 1 << k
            wr = Wre[:, k, :].unsqueeze(2).to_broadcast([128, 16, st])
            wi = Wim[:, k, :].unsqueeze(2).to_broadcast([128, 16, st])
            C.tt("dve", tA, tA[:, :, 0:st], cosT[:, :, 0:st], wr, ALU.mult, [cosT, Wre])
            C.tt("dve", tB, tB[:, :, 0:st], sinT[:, :, 0:st], wi, ALU.mult, [sinT, Wim])
            C.tt("dve", cosT, cosT[:, :, st:2 * st], tA[:, :, 0:st], tB[:, :, 0:st], ALU.subtract, [tA, tB])
            C.tt("dve", tA, tA[:, :, 0:st], cosT[:, :, 0:st], wi, ALU.mult, [cosT, Wim])
            C.tt("dve", tB, tB[:, :, 0:st], sinT[:, :, 0:st], wr, ALU.mult, [sinT, Wre])
            C.tt("dve", sinT, sinT[:, :, st:2 * st], tA[:, :, 0:st], tB[:, :, 0:st], ALU.add, [tA, tB])
        if d == 0:
            dump(C, T, "cosT", cosT[:, :, :].rearrange("p a b -> p (a b)"), cosT)
        C.release(m4)
        car = [sm("car_re"), sm("car_im")]
        C.memset("dve", car[0], car[0][:, :], 0.0)
        C.memset("dve", car[1], car[1][:, :], 0.0)
        zb = [[C.alloc("z%d_%d" % (i, x), (8, TT), F32) for x in range(2)] for i in range(2)]
        sb = [[C.alloc("s%d_%d" % (i, x), (8, TT), BF16) for x in range(2)] for i in range(2)]
        ca, cb_ = C.alloc("ca", (8,), F32), C.alloc("cb", (8,), F32)
        rv = (lambda ap: ap) if d == 0 else (lambda ap: ap[:, :, ::-1])
        order = ([32, 33] + list(range(32))) if d == 0 else list(range(NCH - 1, -1, -1))
        last = TT - 1 if d == 0 else 0
        tp2 = [[C.alloc("tq%d_%d" % (i, k), (4, TT), BF16) for k in range(4)] for i in range(2)]
        cb2 = [[C.alloc("cq%d_%d" % (i, x), (8, TT), BF16) for x in range(2)] for i in range(2)]
        bub = [[C.alloc("bu%d_%d" % (i, x), (4, TT), BF16) for x in range(2)] for i in range(2)]
        cosTb = C.alloc("scosb", (16, TT), BF16)
        sinTb = C.alloc("ssinb", (16, TT), BF16)
        C.copy("dve", cosTb, cosTb[:, :, :], cosT[:, :, :], [cosT])
        C.copy("dve", sinTb, sinTb[:, :, :], sinT[:, :, :], [sinT])
        pp2 = [[C.alloc("pq%d_%d" % (i, x), (8, TT), BF16) for x in range(4)] for i in range(1)]
        zq2 = [[C.alloc("zq%d_%d" % (i, x), (8, TT), BF16) for x in range(2)] for i in range(2)]
        its = [(ch, hf) for ch in order for hf in range(2)]

        def stageA(ch, hf):
            t0 = ch * TT
            psb = C.ps[0:4]
            cbc = C.rot("cb2", cb2)
            for cgl in range(8):
                cg = hf * 8 + cgl
                for x in range(2):
                    bank = psb[x * 2 + cgl // 4]
                    C.mm(bank, bank[:, (cgl % 4) * TT:(cgl % 4 + 1) * TT], WB[:, cg, x, :], uT[:, cg // 4, t0:t0 + TT], True, True,
                         [WB, uT])
            for b in range(2):
                cgs = slice(hf * 8 + b * 4, hf * 8 + b * 4 + 4)
                cl = slice(b * 4, b * 4 + 4)
                cosv = rv(cosTb[:, cgs, :])
                sinv = rv(sinTb[:, cgs, :])
                tpc = C.rot("tp2", tp2)
                bu = C.rot("bub", bub)
                C.copy("act", bu[0], bu[0][:, :, :], psb[b][:, :].rearrange("p (a b) -> p a b", a=4), [psb[b]])
                C.copy("act", bu[1], bu[1][:, :, :], psb[2 + b][:, :].rearrange("p (a b) -> p a b", a=4), [psb[2 + b]])
                bre_ = bu[0][:, :, :]
                bim_ = bu[1][:, :, :]
                C.tt("dve", tpc[0], tpc[0][:, :, :], cosv, bre_, ALU.mult, [cosTb, bu[0]])
                C.tt("dve", tpc[1], tpc[1][:, :, :], sinv, bim_, ALU.mult, [sinTb, bu[1]])
                C.tt("dve", cbc[0], cbc[0][:, cl, :], tpc[0][:, :, :], tpc[1][:, :, :], ALU.add, [tpc[0], tpc[1]])
                C.tt("dve", tpc[2], tpc[2][:, :, :], cosv, bim_, ALU.mult, [cosTb, bu[1]])
                C.tt("dve", tpc[3], tpc[3][:, :, :], sinv, bre_, ALU.mult, [sinTb, bu[0]])
                C.tt("dve", cbc[1], cbc[1][:, cl, :], tpc[2][:, :, :], tpc[3][:, :, :], ALU.subtract, [tpc[2], tpc[3]])
            z = C.rot("zb", zb)
            for cgl in range(8):
                cg = hf * 8 + cgl
                for x in range(2):
                    o_ = z[x][:, cgl, :] if d == 0 else z[x][:, cgl, ::-1]
                    i_ = cbc[x][:, cgl, :] if d == 0 else cbc[x][:, cgl, ::-1]
                    P.op("dve", lambda e, o_=o_, i_=i_, cg=cg, x=x: e.tensor_tensor_scan(
                        out=o_, data0=rmag[:, cg:cg + 1].to_broadcast([128, TT]), data1=i_, initial=car[x][:, cg:cg + 1],
                        op0=ALU.mult, op1=ALU.add), reads=[rmag, cbc[x], car[x]], writes=[z[x]])
            c8 = slice(hf * 8, hf * 8 + 8)
            zr, zi = z[0][:, :, last], z[1][:, :, last]
            C.tt("dve", ca, ca[:, :], Wre[:, 7, c8], zr, ALU.mult, [Wre, z[0]])
            C.tt("dve", cb_, cb_[:, :], Wim[:, 7, c8], zi, ALU.mult, [Wim, z[1]])
            C.tt("dve", car[0], car[0][:, c8], ca[:, :], cb_[:, :], ALU.subtract, [ca, cb_])
            C.tt("dve", ca, ca[:, :], Wre[:, 7, c8], zi, ALU.mult, [Wre, z[1]])
            C.tt("dve", cb_, cb_[:, :], Wim[:, 7, c8], zr, ALU.mult, [Wim, z[0]])
            C.tt("dve", car[1], car[1][:, c8], ca[:, :], cb_[:, :], ALU.add, [ca, cb_])
            return z

        def stageB(ch, hf, z):
            t0 = ch * TT
            c8 = slice(hf * 8, hf * 8 + 8)
            sv = C.rot("sb", sb)
            ppc = C.rot("pp2", pp2)
            c8v = rv(cosTb[:, c8, :])
            s8v = rv(sinTb[:, c8, :])
            zq = C.rot("zq2", zq2)
            C.copy("act", zq[0], zq[0][:, :, :], z[0][:, :, :], [z[0]])
            C.copy("act", zq[1], zq[1][:, :, :], z[1][:, :, :], [z[1]])
            C.tt("dve", ppc[0], ppc[0][:, :, :], c8v, zq[0][:, :, :], ALU.mult, [cosTb, zq[0]])
            C.tt("dve", ppc[1], ppc[1][:, :, :], s8v, zq[1][:, :, :], ALU.mult, [sinTb, zq[1]])
            C.tt("dve", sv[0], sv[0][:, :, :], ppc[0][:, :, :], ppc[1][:, :, :], ALU.subtract, [ppc[0], ppc[1]])
            C.tt("dve", ppc[2], ppc[2][:, :, :], s8v, zq[0][:, :, :], ALU.mult, [sinTb, zq[0]])
            C.tt("dve", ppc[3], ppc[3][:, :, :], c8v, zq[1][:, :, :], ALU.mult, [cosTb, zq[1]])
            C.tt("dve", sv[1], sv[1][:, :, :], ppc[2][:, :, :], ppc[3][:, :, :], ALU.add, [ppc[2], ppc[3]])
            for yl in range(2):
                yt = hf * 2 + yl
                py = C.rot("ps_y", C.ps[4:8])
                for cq in range(4):
                    cg = yt * 4 + cq
                    cgl = cg - hf * 8
                    C.mm(py, py[:, 0:TT], WC[:, cg, 0, :], sv[0][:, cgl, :], cq == 0, False, [WC, sv[0]])
                    C.mm(py, py[:, 0:TT], WC[:, cg, 1, :], sv[1][:, cgl, :], False, cq == 3, [WC, sv[1]])
                if first:
                    C.stt(Yacc, Yacc[:, yt, t0:t0 + TT], uT[:, yt, t0:t0 + TT], dvec[:, yt:yt + 1], py[:, 0:TT], ALU.mult, ALU.add,
                          [uT, dvec, py])
                else:
                    C.tt("dve", Yacc, Yacc[:, yt, t0:t0 + TT], Yacc[:, yt, t0:t0 + TT], py[:, 0:TT], ALU.add, [Yacc, py])

        pend = None
        for (ch, hf) in its:
            z = stageA(ch, hf)
            if pend is not None:
                stageB(*pend)
            pend = (ch, hf, z)
        stageB(*pend)
        dump(C, T, "Yacc%d" % d, Yacc[:, :, :].rearrange("p a b -> p (a b)"), Yacc, BF16)
        C.release(m2)
    Wg = C.alloc("Wglu", (4, 512), BF16)
    P.dma("q_pool", Wg[:, :, :], T["glu_w"][l].rearrange("(k p) c -> p k c", p=128), writes=[Wg])
    gb = C.alloc("glub", (4,), F32)
    load_vec_fm(C, gb, gb[:, :], T["glu_b"][l])
    y2 = C.alloc("gy2", (4, 512), F32)
    sg = C.alloc("gsg", (4, 512), F32)
    ge = [C.alloc("gge%d" % i, (4, 512), BF16) for i in range(2)]
    sg2 = C.alloc("gsg2", (512,), F32)
    yst = [C.alloc("yst%d" % i, (512,), BF16) for i in range(3)]
    tiles = TOK_TILES if need_ctx else TOK_TILES[:8]
    for (t0, n, who) in tiles:
        y = Yacc[:, :, t0:t0 + n]
        g_ = C.rot("gge", ge)
        C.tt("pool", y2, y2[:, :, 0:n], y, y, ALU.mult, [Yacc])
        C.ts("pool", y2, y2[:, :, 0:n], y2[:, :, 0:n], 0.044715, ALU.mult, [y2], s2=1.0, op1=ALU.add)
        C.tt("pool", y2, y2[:, :, 0:n], y2[:, :, 0:n], y, ALU.mult, [y2, Yacc])
        C.act(sg, sg[:, :, 0:n], y2[:, :, 0:n], AF.Sigmoid, [y2], scale=1.5957691216057308)
        C.tt("dve", g_, g_[:, :, 0:n], y, sg[:, :, 0:n], ALU.mult, [Yacc, sg])
        for ob in range(4):
            ps = C.rot("ps_g", C.ps[0:4])
            for k in range(4):
                C.mm(ps, ps[:, 0:n], Wg[:, k, ob * 128:(ob + 1) * 128], g_[:, k, 0:n], k == 0, k == 3, [Wg, g_])
            C.act(sg2, sg2[:, 0:n], ps[:, 0:n], AF.Sigmoid, [ps, gb], bias=gb[:, ob:ob + 1])
            st = C.rot("yst", yst)
            C.tt("dve", st, st[:, 0:n], g_[:, ob, 0:n], sg2[:, 0:n], ALU.mult, [g_, sg2])
            P.dma("q_sp", T["YST"][ob * 128:(ob + 1) * 128, t0:t0 + n], st[:, 0:n], reads=[st], writes=[T["YST_b"]])
    C.release(m)


def phase_merge(C, T, l, need_ctx):
    P = C.P
    MOD = T["MOD"]
    m = C.mark()
    Wb = []
    for i, nm in enumerate(("w_branch_ssm", "w_branch_gqa", "w_branch_na")):
        w = C.alloc("Wb%d" % i, (4, 1024), BF16)
        P.dma("q_pool", w[:, :, :], T[nm][l].rearrange("(k p) c -> p k c", p=128), writes=[w])
        Wb.append(w)
    Wo = C.alloc("Wo", (8, 1024), BF16)
    P.dma("q_pool", Wo[:, :, :], T["w_out"][l].rearrange("(k p) c -> p k c", p=128), writes=[Wo])
    Ys = [[C.alloc("my%d_%d" % (i, br), (4, 512), BF16) for br in range(3)] for i in range(2)]
    Gs = [C.alloc("mg%d" % i, (24, 512), BF16) for i in range(2)]
    xts = [C.alloc("mx%d" % i, (8, 512), F32) for i in range(2)]
    mT = C.alloc("mT", (8, 512), BF16)
    tA = [C.alloc("mtA%d" % i, (512,), F32) for i in range(2)]
    tB = [C.alloc("mtB%d" % i, (512,), F32) for i in range(2)]
    XTv = T["XT"].rearrange("(k p) t -> p k t", p=128)
    srcs = [T["YST"], T["YGT"], T["YNT"]]
    srcb = [T["YST_b"], T["YGT_b"], T["YNT_b"]]
    tiles = TOK_TILES if need_ctx else TOK_TILES[:8]
    for (t0, n, who) in tiles:
        Y = C.rot("mY", Ys)
        G = C.rot("mG", Gs)
        xt = C.rot("mx", xts)
        for br in range(3):
            P.dma("q_sp", Y[br][:, :, 0:n], srcs[br].rearrange("(k p) t -> p k t", p=128)[:, :, t0:t0 + n], reads=[srcb[br]],
                  writes=[Y[br]])
        P.dma("q_act", G[:, :, 0:n], T["GT"].rearrange("(k p) t -> p k t", p=128)[:, :, t0:t0 + n], reads=[T["GT_b"]], writes=[G])
        P.dma("q_sp", xt[:, :, 0:n], XTv[:, :, t0:t0 + n], reads=[T["XT_b"]], writes=[xt])
        for ob in range(8):
            a = C.rot("mtA", tA)
            for br in range(3):
                ps = C.rot("ps_m", C.ps[0:4])
                for k in range(4):
                    C.mm(ps, ps[:, 0:n], Wb[br][:, k, ob * 128:(ob + 1) * 128], Y[br][:, k, 0:n], k == 0, k == 3, [Wb[br], Y[br]])
                if br == 0:
                    C.tt("dve", a, a[:, 0:n], ps[:, 0:n], G[:, ob, 0:n], ALU.mult, [ps, G])
                else:
                    b = C.rot("mtB", tB)
                    C.tt("dve", b, b[:, 0:n], ps[:, 0:n], G[:, br * 8 + ob, 0:n], ALU.mult, [ps, G])
                    if br == 1:
                        C.tt("pool", a, a[:, 0:n], a[:, 0:n], b[:, 0:n], ALU.add, [a, b])
                    else:
                        C.tt("pool", mT, mT[:, ob, 0:n], a[:, 0:n], b[:, 0:n], ALU.add, [a, b])
        for ob in range(8):
            ps = C.rot("ps_m2", C.ps[4:8])
            for k in range(8):
                C.mm(ps, ps[:, 0:n], Wo[:, k, ob * 128:(ob + 1) * 128], mT[:, k, 0:n], k == 0, k == 7, [Wo, mT])
            C.stt(xt, xt[:, ob, 0:n], ps[:, 0:n], MOD[:, 16 + ob, who:who + 1], xt[:, ob, 0:n], ALU.mult, ALU.add, [ps, MOD, xt])
        P.dma("q_act", XTv[:, :, t0:t0 + n], xt[:, :, 0:n], reads=[xt], writes=[T["XT_b"]])
    C.release(m)


def phase_ffn(C, T, l, need_ctx):
    P = C.P
    MOD = T["MOD"]
    moe = (l % 2 == 1)
    j = l // 2
    if moe:
        experts = [(T["moe_w_gate"][j, e], T["moe_w_up"][j, e], T["moe_w_down"][j, e]) for e in range(NE)]
        chunks = [(b, 4) for b in range(0, 28, 4)]
    else:
        experts = [(T["ffn_w_gate"][j], T["ffn_w_up"][j], T["ffn_w_down"][j])]
        chunks = [(b, 4) for b in range(0, 20, 4)] + [(20, 2)]
    m = C.mark()
    h2 = C.alloc("h2", (8, 1024), BF16)
    oacc = C.alloc("oacc", (8, 1024), F32)
    wgs = [C.alloc("wg%d" % i, (8, 512), BF16) for i in range(2)]
    wus = [C.alloc("wu%d" % i, (8, 512), BF16) for i in range(2)]
    wds = [C.alloc("wd%d" % i, (4, 1024), BF16) for i in range(3)]
    xt = C.alloc("fx", (8, 256), F32)
    sq = C.alloc("fsq", (8, 256), F32)
    tmp = C.alloc("ftmp", (8, 256), F32)
    rstd = C.alloc("frstd", (256,), F32)
    actb = [C.alloc("actb%d" % i, (4, 512), BF16) for i in range(2)]
    sil = [C.alloc("sil%d" % i, (512,), F32) for i in range(2)]
    silg = [C.alloc("silg%d" % i, (512,), F32) for i in range(2)]
    if moe:
        h2f = C.alloc("h2f", (8, 256), F32)
        GB = C.alloc("GB", (8, 1024), BF16)
        rw = C.alloc("rw", (8, 8), F32)
        P.dma("q_sp", rw[:, :, :], T["router_w"][j].rearrange("(k p) e -> p k e", p=128), writes=[rw])
        sm = lambda nm, n: C.alloc(nm, (n,), F32)
        L, eq, L2, sel, ex, wv, gts = [sm(x, 8) for x in ("rL", "req", "rL2", "rsel", "rex", "rwv", "rgts")]
        m1, m2, nm1, den = [sm(x, 1) for x in ("rm1", "rm2", "rnm1", "rden")]
        gbl = [C.alloc("gbl%d" % i, (128,), F32) for i in range(2)]
    XTv = T["XT"].rearrange("(k p) t -> p k t", p=128)
    sts = [(i * 1024, 1024, 0) for i in range(4)] + ([(NL, 256, 1)] if need_ctx else [])
    for (ts0, TS, who) in sts:
        nsub = TS // 256
        for sub in range(nsub):
            t0 = ts0 + sub * 256
            cs0 = sub * 256
            P.dma("q_sp", xt[:, :, :], XTv[:, :, t0:t0 + 256], reads=[T["XT_b"]], writes=[xt])
            outs = [(h2, lambda k, cs0=cs0: h2[:, k, cs0:cs0 + 256])]
            if moe:
                outs.append((h2f, lambda k: h2f[:, k, :]))
            norm_modulate(C, T, xt, 256, who, T["A2"], 3, outs, tmp, sq, rstd, C.ps[7])
            if moe:
                for tt_ in range(2):
                    pl = C.ps[6]
                    for k in range(8):
                        C.mm(pl, pl[:, 0:8], h2f[:, k, tt_ * 128:(tt_ + 1) * 128], rw[:, k, :], k == 0, k == 7, [h2f, rw])
                    C.copy("act", L, L[:, :], pl[:, 0:8], [pl])
                    P.op("dve", lambda e: e.tensor_reduce(out=m1[:, :], in_=L[:, :], axis=AX.X, op=ALU.max), reads=[L], writes=[m1])
                    C.ts("dve", eq, eq[:, :], L[:, :], m1[:, 0:1], ALU.is_equal, [L, m1])
                    C.stt(L2, L2[:, :], eq[:, :], -1e30, L[:, :], ALU.mult, ALU.add, [eq, L])
                    P.op("dve", lambda e: e.tensor_reduce(out=m2[:, :], in_=L2[:, :], axis=AX.X, op=ALU.max), reads=[L2], writes=[m2])
                    C.ts("dve", sel, sel[:, :], L[:, :], m2[:, 0:1], ALU.is_ge, [L, m2])
                    C.ts("dve", nm1, nm1[:, :], m1[:, :], -1.0, ALU.mult, [m1])
                    C.act(ex, ex[:, :], L[:, :], AF.Exp, [L, nm1], bias=nm1[:, 0:1])
                    C.tt("dve", wv, wv[:, :], ex[:, :], sel[:, :], ALU.mult, [ex, sel])
                    P.op("dve", lambda e: e.tensor_reduce(out=den[:, :], in_=wv[:, :], axis=AX.X, op=ALU.add), reads=[wv], writes=[den])
                    C.recip(den, den[:, :], den[:, :], [den])
                    C.ts("dve", gts, gts[:, :], wv[:, :], den[:, 0:1], ALU.mult, [wv, den])
                    for e4 in range(2):
                        pg_ = C.rot("ps_gb", C.ps[4:6])
                        for ee in range(4):
                            e_ = e4 * 4 + ee
                            gb_ = C.rot("gbl", gbl)
                            C.copy("dve", gb_, gb_[:, :], gts[:, e_:e_ + 1].to_broadcast([128, 128]), [gts])
                            C.mm(pg_, pg_[:, ee * 128:(ee + 1) * 128], gb_[:, :], T["identF"][:, :], True, True, [gb_, T["identF_b"]])
                        pos = cs0 + tt_ * 128
                        C.copy("act", GB, GB[:, e4 * 4:(e4 + 1) * 4, pos:pos + 128],
                               pg_[:, :].rearrange("p (a b) -> p a b", a=4), [pg_])
        SW = min(512, TS)
        nsw = TS // SW
        pend = None

        def down(ab, wd, nb, cs, first):
            for ob in range(8):
                po = C.rot("ps_fo", C.ps[4:6])
                for hb in range(nb):
                    C.mm(po, po[:, 0:SW], wd[:, hb, ob * 128:(ob + 1) * 128], ab[:, hb, 0:SW], hb == 0, hb == nb - 1, [wd, ab])
                if first:
                    C.copy("act", oacc, oacc[:, ob, cs], po[:, 0:SW], [po])
                else:
                    C.tt("dve", oacc, oacc[:, ob, cs], oacc[:, ob, cs], po[:, 0:SW], ALU.add, [oacc, po])

        for ei, (wg_ap, wu_ap, wd_ap) in enumerate(experts):
            wgv = wg_ap.rearrange("(k p) c -> p k c", p=128)
            wuv = wu_ap.rearrange("(k p) c -> p k c", p=128)
            wdv = wd_ap.rearrange("(hb p) c -> p hb c", p=128)
            for (b0, nb) in chunks:
                wg, wu, wd = C.rot("wgs", wgs), C.rot("wus", wus), C.rot("wds", wds)
                P.dma("q_pool", wg[:, :, 0:nb * 128], wgv[:, :, b0 * 128:(b0 + nb) * 128], writes=[wg])
                P.dma("q_pool", wu[:, :, 0:nb * 128], wuv[:, :, b0 * 128:(b0 + nb) * 128], writes=[wu])
                P.dma("q_pool", wd[:, 0:nb, :], wdv[:, b0:b0 + nb, :], writes=[wd])
                first = (ei == 0 and b0 == 0)
                for sub in range(nsw):
                    cs = slice(sub * SW, (sub + 1) * SW)
                    ab = C.rot("actb", actb)
                    for hb in range(nb):
                        pg = C.rot("ps_fg", C.ps[0:2])
                        pu = C.rot("ps_fu", C.ps[2:4])
                        for k in range(8):
                            C.mm(pg, pg[:, 0:SW], wg[:, k, hb * 128:(hb + 1) * 128], h2[:, k, cs], k == 0, k == 7, [wg, h2])
                        for k in range(8):
                            C.mm(pu, pu[:, 0:SW], wu[:, k, hb * 128:(hb + 1) * 128], h2[:, k, cs], k == 0, k == 7, [wu, h2])
                        sl_ = C.rot("sil", sil)
                        C.act(sl_, sl_[:, 0:SW], pg[:, 0:SW], AF.Silu, [pg])
                        if moe:
                            sg_ = C.rot("silg", silg)
                            C.tt("pool", sg_, sg_[:, 0:SW], sl_[:, 0:SW], GB[:, ei, cs], ALU.mult, [sl_, GB])
                            C.tt("dve", ab, ab[:, hb, 0:SW], pu[:, 0:SW], sg_[:, 0:SW], ALU.mult, [pu, sg_])
                        else:
                            C.tt("dve", ab, ab[:, hb, 0:SW], pu[:, 0:SW], sl_[:, 0:SW], ALU.mult, [pu, sl_])
                    if pend is not None:
                        down(*pend)
                    pend = (ab, wd, nb, cs, first)
        down(*pend)
        for sub in range(nsub):
            t0 = ts0 + sub * 256
            cs = slice(sub * 256, (sub + 1) * 256)
            P.dma("q_sp", xt[:, :, :], XTv[:, :, t0:t0 + 256], reads=[T["XT_b"]], writes=[xt])
            for ob in range(8):
                C.stt(xt, xt[:, ob, :], oacc[:, ob, cs], MOD[:, 40 + ob, who:who + 1], xt[:, ob, :], ALU.mult, ALU.add, [oacc, MOD, xt])
            P.dma("q_act", XTv[:, :, t0:t0 + 256], xt[:, :, :], reads=[xt], writes=[T["XT_b"]])
    C.release(m)


def phase_final(C, T):
    P = C.P
    m = C.mark()
    gf = C.alloc("gf", (8,), F32)
    load_vec_fm(C, gf, gf[:, :], T["final_norm_g"])
    xts = [C.alloc("ox%d" % i, (8, 512), F32) for i in range(2)]
    sq = C.alloc("osq", (8, 512), F32)
    rstd = C.alloc("orstd", (512,), F32)
    yv = C.alloc("oy", (8, 512), F32)
    ots = [C.alloc("ot%d" % i, (1024,), F32) for i in range(2)]
    XTv = T["XT"].rearrange("(k p) t -> p k t", p=128)
    for (t0, n, who) in TOK_TILES[:8]:
        xt = C.rot("ox", xts)
        P.dma("q_sp", xt[:, :, :], XTv[:, :, t0:t0 + n], reads=[T["XT_b"]], writes=[xt])
        C.act(sq, sq[:, :, :], xt[:, :, :], AF.Square, [xt])
        pss = C.ps[7]
        for k in range(8):
            C.mm(pss, pss[:, 0:n], T["onesF"][:, :], sq[:, k, :], k == 0, k == 7, [sq, T["onesF_b"]])
        C.act(rstd, rstd[:, :], pss[:, 0:n], AF.Sqrt, [pss], scale=1.0 / D, bias=T["eps"][:, 0:1])
        C.recip(rstd, rstd[:, :], rstd[:, :], [rstd])
        for k in range(8):
            C.stt(yv, yv[:, k, :], xt[:, k, :], gf[:, k:k + 1], rstd[:, :], ALU.mult, ALU.mult, [xt, gf, rstd])
        for blk in range(n // 128):
            ot = C.rot("ot", ots)
            for half in range(2):
                ps = C.rot("ps_o", C.ps[0:4])
                for kk in range(4):
                    k = half * 4 + kk
                    C.mm(ps, ps[:, kk * 128:(kk + 1) * 128], yv[:, k, blk * 128:(blk + 1) * 128], T["identF"][:, :], True, True,
                         [yv, T["identF_b"]])
                C.copy("act" if half == 0 else "dve", ot, ot[:, half * 512:(half + 1) * 512], ps[:, :], [ps])
            P.dma("q_act", T["out"][t0 + blk * 128:t0 + (blk + 1) * 128, :], ot[:, :], reads=[ot])
    C.release(m)


def host_consts():
    cst = {}
    cst["identF"] = np.eye(128, dtype=np.float32)
    cst["onesF"] = np.ones((128, 128), np.float32)
    b = np.zeros((128, 128), np.float32)
    b[:64, :64] = 1.0
    b[64:, 64:] = 1.0
    cst["blk64F"] = b
    R = np.zeros((128, 128), np.float32)
    for h in range(2):
        for half in range(2):
            o = h * 64 + half * 32
            for j in range(16):
                R[o + j + 16, o + j] = -1.0
                R[o + j, o + j + 16] = 1.0
    cst["rotM"] = R.astype(ml_dtypes.bfloat16)
    t = np.arange(NL)
    pos = np.stack([t // GRID_W, t % GRID_W], -1).astype(np.float32)
    half = 32
    inv = (1.0 / (10000.0 ** (np.arange(0, half, 2, dtype=np.float32) / half))).astype(np.float32)
    ang = pos[:, :, None] * inv
    ang = np.concatenate([ang, ang], -1).reshape(NL, 64)
    cosv = np.concatenate([np.cos(ang).astype(np.float32), np.ones((NC_, 64), np.float32)], 0)
    sinv = np.concatenate([np.sin(ang).astype(np.float32), np.zeros((NC_, 64), np.float32)], 0)
    cst["cos"] = np.ascontiguousarray(np.concatenate([cosv.T, cosv.T], 0)).astype(ml_dtypes.bfloat16)
    cst["sin"] = np.ascontiguousarray(np.concatenate([sinv.T, sinv.T], 0)).astype(ml_dtypes.bfloat16)
    qc = np.arange(64)
    cs = np.clip(qc - 8, 0, 48)
    kc = np.arange(64)[:, None]
    valid = (kc >= cs[None, :]) & (kc < cs[None, :] + 16)
    mk = np.where(valid, 0.0, -240000.0).astype(np.float32)
    mk = np.broadcast_to(mk[None, :, None, None, :], (2, 64, 8, 14, 64)).reshape(128, 8, 14, 64)
    cst["namask"] = np.ascontiguousarray(mk).astype(ml_dtypes.bfloat16)
    rmk = np.zeros((128, 16), np.float32)
    for k in range(8):
        rmk[k * 16:(k + 1) * 16, k] = 1.0
        rmk[k * 16:(k + 1) * 16, 8 + k] = -1.0
    cst["rowmask"] = rmk
    return cst


def na_rpb_gather(rpb):
    kc = np.arange(64)[:, None]
    qc = np.arange(64)[None, :]
    idx = np.clip(kc - qc + 15, 0, 30)
    return np.ascontiguousarray(rpb[:, :, :, idx])


CONST_DT = {"identF": F32, "onesF": F32, "blk64F": F32, "rotM": BF16, "cos": BF16, "sin": BF16, "namask": BF16, "rowmask": F32}

WEIGHT_SPECS = [
    ("c_ctx", [D]), ("w_mod", [DEPTH, D, 6 * D]), ("b_mod", [DEPTH, 6 * D]), ("norm1_g", [DEPTH, D]),
    ("w_in", [DEPTH, D, IN_COLS]),
    ("ssm_a_re", [DEPTH, 2, 32, 64]), ("ssm_a_im", [DEPTH, 2, 32, 64]),
    ("ssm_b_re", [DEPTH, 2, 32, 64, 16]), ("ssm_b_im", [DEPTH, 2, 32, 64, 16]),
    ("ssm_c_re", [DEPTH, 2, 32, 16, 64]), ("ssm_c_im", [DEPTH, 2, 32, 16, 64]),
    ("ssm_log_dt", [DEPTH, 2, 32]), ("ssm_d", [DEPTH, 512]), ("glu_w", [DEPTH, 512, 512]), ("glu_b", [DEPTH, 512]),
    ("q_norm_g", [DEPTH, 64]), ("k_norm_g", [DEPTH, 64]), ("na_rpb", [DEPTH, 8, 15, 31]),
    ("w_branch_ssm", [DEPTH, 512, D]), ("w_branch_gqa", [DEPTH, 512, D]), ("w_branch_na", [DEPTH, 512, D]),
    ("w_out", [DEPTH, D, D]), ("norm2_g", [DEPTH, D]),
    ("ffn_w_gate", [1, D, FFN_DIM]), ("ffn_w_up", [1, D, FFN_DIM]), ("ffn_w_down", [1, FFN_DIM, D]),
    ("router_w", [1, D, NE]), ("moe_w_gate", [1, NE, D, EXPERT_DIM]), ("moe_w_up", [1, NE, D, EXPERT_DIM]),
    ("moe_w_down", [1, NE, EXPERT_DIM, D]), ("final_norm_g", [D]),
]


def build_program(stop=None, debug=()):
    nc = bass.Bass("TRN2", target_bir_lowering=False)
    wspecs = dict(WEIGHT_SPECS)

    class LazyT(dict):
        def __missing__(self, name):
            if name in wspecs:
                ap = nc.dram_tensor(name, wspecs[name], F32, kind="ExternalInput").ap()
                self[name] = ap
                self["_used"].append(name)
                return ap
            raise KeyError(name)

    T = LazyT()
    T["_used"] = []
    T["_debug"] = tuple(debug)
    T["x"] = nc.dram_tensor("x", [NL, D], F32, kind="ExternalInput").ap()
    T["ctx"] = nc.dram_tensor("ctx", [NC_, D], F32, kind="ExternalInput").ap()
    T["c"] = nc.dram_tensor("c", [D], F32, kind="ExternalInput").ap()
    cst = host_consts()
    cdram = {}
    for k, v in cst.items():
        cdram[k] = nc.dram_tensor("k_" + k, list(v.shape), CONST_DT[k], kind="ExternalInput").ap()
    T["out"] = nc.dram_tensor("out", [NL, D], F32, kind="ExternalOutput").ap()
    T["rpbT"] = nc.dram_tensor("rpbT", [DEPTH, 8, 15, 64, 64], F32, kind="ExternalInput").ap()
    T["namask"] = cdram["namask"]
    T["rowmask"] = cdram["rowmask"]

    def scratch(name, shape, dt):
        kind = "ExternalOutput" if name in debug else "Internal"
        T[name] = nc.dram_tensor("s_" + name, shape, dt, kind=kind).ap()
        T[name + "_b"] = Buf(name)

    scratch("XT", [D, LT], F32)
    scratch("UT", [512, LT], BF16)
    scratch("KGT", [128, LT], BF16)
    scratch("VG", [LT, 128], BF16)
    scratch("KNT", [512, LT], BF16)
    scratch("VN", [LT, 512], BF16)
    scratch("QGT", [512, LT], BF16)
    scratch("QNT", [512, LT], BF16)
    scratch("GT", [3072, LT], BF16)
    scratch("YST", [512, LT], BF16)
    scratch("YGT", [512, LT], BF16)
    scratch("YNT", [512, LT], BF16)

    C = Ctx(nc)
    P = C.P
    for k in ("identF", "onesF", "blk64F"):
        b = C.alloc(k, (128,), F32)
        P.dma("q_sp", b[:, :], cdram[k], writes=[b])
        T[k] = b
        T[k + "_b"] = b
    b = C.alloc("rotM", (128,), BF16)
    P.dma("q_sp", b[:, :], cdram["rotM"], writes=[b])
    T["rotM"] = b
    T["rotM_b"] = b
    T["cos"] = cdram["cos"]
    T["sin"] = cdram["sin"]
    T["eps"] = C.alloc("eps", (1,), F32)
    C.memset("dve", T["eps"], T["eps"][:, :], EPS)
    T["MOD"] = C.alloc("MOD", (48, 2), F32)
    T["A1"] = C.alloc("A1", (8, 2), F32)
    T["A2"] = C.alloc("A2", (8, 2), F32)

    def done():
        P.barrier()
        P.emit()
        return nc, cst, list(T["_used"])

    phase_setup(C, T)
    if stop == "setup":
        return done()
    for l in range(DEPTH):
        phase_mod(C, T, l)
        if stop == "mod%d" % l:
            return done()
        phase_inproj(C, T, l)
        if stop == "inproj%d" % l:
            return done()
        need_ctx = l < DEPTH - 1
        if "skip_gqa" not in debug:
            phase_gqa(C, T, l, need_ctx)
        if stop == "gqa%d" % l:
            return done()
        if "skip_na" not in debug:
            phase_na(C, T, l, need_ctx)
        if stop == "na%d" % l:
            return done()
        if "skip_ssm" not in debug:
            phase_ssm(C, T, l, need_ctx)
        if stop == "ssm%d" % l:
            return done()
        phase_merge(C, T, l, need_ctx)
        if stop == "merge%d" % l:
            return done()
        phase_ffn(C, T, l, need_ctx)
        if stop == "ffn%d" % l:
            return done()
    phase_final(C, T)
    return done()


def make_in_maps(inputs, cst, used, n_cores=8):
    maps = []
    rpbT = na_rpb_gather(np.asarray(inputs["na_rpb"]))
    for b in range(n_cores):
        m = {"x": np.ascontiguousarray(inputs["x"][b]), "ctx": np.ascontiguousarray(inputs["ctx"][b]),
             "c": np.ascontiguousarray(inputs["c"][b])}
        for name in used:
            m[name] = np.asarray(inputs[name])
        for k, v in cst.items():
            m["k_" + k] = v
        m["rpbT"] = rpbT
        maps.append(m)
    return maps


def kernel(**inputs):
    nc, cst, used = build_program()
    maps = make_in_maps(inputs, cst, used, 8)
    res = run_bass_kernel_spmd(nc, maps, core_ids=list(range(8)))
    return np.stack([r["out"] for r in res.results], 0).astype(np.float32)
```

```python
import contextlib
import math
import numpy as np
import ml_dtypes
import concourse.bass as bass
import concourse.mybir as mybir
from concourse.bass_utils import run_bass_kernel_spmd

F32 = mybir.dt.float32
BF16 = mybir.dt.bfloat16
U8 = mybir.dt.uint8
AF = mybir.ActivationFunctionType
ALU = mybir.AluOpType
AX = mybir.AxisListType

SAME_ENGINE_RAW_SYNC = True
SSM_ENG = "dve"
N_DMA_SEMS = 8

D = 1024
NL = 4096
NC_ = 256
LT = NL + NC_
DEPTH = 2
IN_COLS = 5888
FFN_DIM = 2816
NE = 8
EXPERT_DIM = 3584
EPS = 1e-6
GRID_W = 64


class Buf:
    __slots__ = ("name", "t", "writer", "readers")

    def __init__(self, name, t=None):
        self.name = name
        self.t = t
        self.writer = None
        self.readers = []

    def __getitem__(self, k):
        return self.t[k]


class Op:
    __slots__ = ("stream", "fn", "waits", "signal", "sigval", "is_dma")

    def __init__(self, stream, fn, is_dma):
        self.stream = stream
        self.fn = fn
        self.waits = {}
        self.signal = False
        self.sigval = None
        self.is_dma = is_dma


class Prog:
    COMPUTE = ("pe", "act", "dve", "pool")
    QUEUES = {"q_sp": "sp", "q_act": "act", "q_pool": "pool"}

    def __init__(self, nc):
        self.nc = nc
        self.ops = {s: [] for s in ("pe", "act", "dve", "pool", "sp")}
        self.count = {s: 0 for s in self.COMPUTE}
        self.dcount = {q: 0 for q in self.QUEUES}
        self.stack = contextlib.ExitStack()
        self.sems = {s: self.stack.enter_context(nc.semaphore("s_" + s)) for s in self.COMPUTE}
        self.dsems = {q: [self.stack.enter_context(nc.semaphore("d_%s_%d" % (q, i))) for i in range(N_DMA_SEMS)]
                      for q in self.QUEUES}
        self.allops = {}
        self.order = {s: [] for s in self.COMPUTE}

    def _host(self, stream):
        return self.QUEUES.get(stream, stream)

    def _add(self, stream, fn, reads, writes):
        is_dma = stream in self.QUEUES
        host = self._host(stream)
        op = Op(stream, fn, is_dma)
        if is_dma:
            idx = self.dcount[stream]
            self.dcount[stream] += 1
            key = (stream, idx)
            if idx >= N_DMA_SEMS:
                self._dep(op, (stream, idx - N_DMA_SEMS))
        else:
            idx = self.count[stream]
            self.count[stream] += 1
            key = (stream, idx)
            self.order[stream].append(op)
        self.allops[key] = op
        for b in reads:
            if b.writer is not None:
                self._dep(op, b.writer, raw=True)
        for b in writes:
            if b.writer is not None:
                self._dep(op, b.writer)
            for r in b.readers:
                self._dep(op, r)
        for b in reads:
            b.readers.append(key)
        for b in writes:
            b.writer = key
            b.readers = []
        self.ops[host].append(op)
        return op

    def _dep(self, op, key, raw=False):
        pstream, pidx = key
        if pstream in self.QUEUES:
            wkey = ("d", pstream, pidx % N_DMA_SEMS)
            val = 16 * (pidx // N_DMA_SEMS + 1)
        else:
            if pstream == op.stream and not (raw and SAME_ENGINE_RAW_SYNC and pstream != "pe"):
                return
            self.allops[key].signal = True
            wkey = ("c", pstream)
            val = pidx
        cur = op.waits.get(wkey)
        if cur is None or val > cur:
            op.waits[wkey] = val

    def op(self, stream, fn, reads=(), writes=()):
        return self._add(stream, fn, list(reads), list(writes))

    def dma(self, q, out, in_, reads=(), writes=(), **kw):
        return self._add(q, lambda e: e.dma_start(out=out, in_=in_, **kw), list(reads), list(writes))

    def barrier(self):
        lasts = []
        for s in self.COMPUTE:
            if self.count[s] > 0:
                lasts.append((s, self.count[s] - 1))
        for q in self.QUEUES:
            for i in range(max(0, self.dcount[q] - N_DMA_SEMS), self.dcount[q]):
                lasts.append((q, i))
        for host in ("pe", "act", "dve", "pool", "sp"):
            op = Op(host, (lambda e: e.nop()), False)
            if host in self.COMPUTE:
                idx = self.count[host]
                self.count[host] += 1
                self.allops[(host, idx)] = op
                self.order[host].append(op)
            for key in lasts:
                if key[0] == host:
                    continue
                self._dep(op, key)
            self.ops[host].append(op)

    def emit(self):
        nc = self.nc
        order = self.order
        for s in self.COMPUTE:
            c = 0
            for op in order[s]:
                if op.signal:
                    c += 1
                    op.sigval = c
        prog = self

        def run(host, e):
            waited = {}
            dq = {q: 0 for q in prog.QUEUES}
            for op in prog.ops[host]:
                for wkey, val in op.waits.items():
                    if wkey[0] == "d":
                        sem = prog.dsems[wkey[1]][wkey[2]]
                        v = val
                    else:
                        sem = prog.sems[wkey[1]]
                        v = order[wkey[1]][val].sigval
                    if waited.get(wkey, 0) >= v:
                        continue
                    waited[wkey] = v
                    e.wait_ge(sem, v)
                ins = op.fn(e)
                if op.is_dma:
                    i = dq[op.stream]
                    dq[op.stream] += 1
                    ins.then_inc(prog.dsems[op.stream][i % N_DMA_SEMS], 16)
                elif op.signal:
                    ins.then_inc(prog.sems[op.stream], 1)

        with nc.Block() as block:
            @block.tensor
            def _(e):
                run("pe", e)

            @block.scalar
            def _(e):
                run("act", e)

            @block.vector
            def _(e):
                run("dve", e)

            @block.gpsimd
            def _(e):
                run("pool", e)

            @block.sync
            def _(e):
                run("sp", e)
        self.stack.close()


class Ctx:
    def __init__(self, nc):
        self.nc = nc
        self.P = Prog(nc)
        self.cap = 200 * 1024
        self.big = nc.alloc_sbuf_tensor("big", [128, self.cap], U8)
        self.off = 0
        self.ps = [Buf("ps%d" % i, nc.alloc_psum_tensor("ps%d" % i, [128, 512], F32)) for i in range(8)]
        self.rr = {}

    def alloc(self, name, shape, dt):
        n = 1
        for s in shape:
            n *= s
        sz = n * (4 if dt == F32 else 2)
        sz = (sz + 63) // 64 * 64
        assert self.off + sz <= self.cap, "SBUF overflow %s: %d + %d" % (name, self.off, sz)
        ap = self.big[:, self.off:self.off + n * (4 if dt == F32 else 2)].bitcast(dt)
        self.off += sz
        if len(shape) == 2:
            ap = ap.rearrange("p (a b) -> p a b", a=shape[0])
        elif len(shape) == 3:
            ap = ap.rearrange("p (a b c) -> p a b c", a=shape[0], b=shape[1])
        return Buf(name, ap)

    def mark(self):
        return self.off

    def release(self, m):
        self.P.barrier()
        self.off = m

    def rot(self, key, lst):
        i = self.rr.get(key, 0)
        self.rr[key] = i + 1
        return lst[i % len(lst)]

    def mm(self, ps, out, lhsT, rhs, start, stop, reads, tp=None):
        kw = {}
        if tp is not None:
            kw["tile_position"] = tp
        return self.P.op("pe", lambda e: e.matmul(out, lhsT=lhsT, rhs=rhs, start=start, stop=stop, **kw),
                         reads=reads, writes=[ps])

    def act(self, ob, out, in_, func, reads, scale=1.0, bias=0.0):
        return self.P.op("act", lambda e: e.activation(out=out, in_=in_, func=func, bias=bias, scale=scale),
                         reads=reads, writes=[ob])

    def tt(self, eng, ob, out, in0, in1, op, reads):
        return self.P.op(eng, lambda e: e.tensor_tensor(out=out, in0=in0, in1=in1, op=op), reads=reads, writes=[ob])

    def ts(self, eng, ob, out, in0, s1, op0, reads, s2=None, op1=None):
        if op1 is None:
            return self.P.op(eng, lambda e: e.tensor_scalar(out=out, in0=in0, scalar1=s1, scalar2=None, op0=op0),
                             reads=reads, writes=[ob])
        return self.P.op(eng, lambda e: e.tensor_scalar(out=out, in0=in0, scalar1=s1, scalar2=s2, op0=op0, op1=op1),
                         reads=reads, writes=[ob])

    def stt(self, ob, out, in0, scalar, in1, op0, op1, reads):
        return self.P.op("dve", lambda e: e.scalar_tensor_tensor(out=out, in0=in0, scalar=scalar, in1=in1, op0=op0, op1=op1),
                         reads=reads, writes=[ob])

    def copy(self, eng, ob, out, in_, reads):
        if eng == "act":
            return self.P.op("act", lambda e: e.copy(out=out, in_=in_), reads=reads, writes=[ob])
        return self.P.op(eng, lambda e: e.tensor_copy(out=out, in_=in_), reads=reads, writes=[ob])

    def recip(self, ob, out, in_, reads):
        return self.P.op("dve", lambda e: e.reciprocal(out=out, in_=in_), reads=reads, writes=[ob])

    def memset(self, eng, ob, out, val):
        return self.P.op(eng, lambda e: e.memset(out, val), reads=[], writes=[ob])


def dump(C, T, name, ap, buf, dt=None):
    if name not in T.get("_debug", ()):
        return
    d = C.nc.dram_tensor("dbg_" + name, list(ap.shape), dt or F32, kind="ExternalOutput").ap()
    C.P.dma("q_sp", d, ap, reads=[buf])


TOK_TILES = [(i * 512, 512, 0) for i in range(8)] + [(NL, 256, 1)]


def phase_setup(C, T):
    P = C.P
    m = C.mark()
    xin = [C.alloc("xin%d" % i, (1024,), F32) for i in range(2)]
    xst = [C.alloc("xst%d" % i, (8, 128), F32) for i in range(2)]
    XTv = T["XT"].rearrange("(k p) t -> p k t", p=128)
    for tt in range(LT // 128):
        t0 = tt * 128
        xi = C.rot("xin", xin)
        st = C.rot("xst", xst)
        src = T["x"][t0:t0 + 128, :] if t0 < NL else T["ctx"][t0 - NL:t0 - NL + 128, :]
        P.dma("q_sp", xi[:, :], src, writes=[xi])
        for half in range(2):
            ps = C.rot("ps_setup", C.ps[0:4])
            for kk in range(4):
                k = half * 4 + kk
                C.mm(ps, ps[:, kk * 128:(kk + 1) * 128], xi[:, k * 128:(k + 1) * 128], T["identF"][:, :], True, True,
                     [xi, T["identF_b"]])
            eng = "act" if half == 0 else "dve"
            C.copy(eng, st, st[:, half * 4:(half + 1) * 4, :], ps[:, :].rearrange("p (a b) -> p a b", a=4), [ps])
        P.dma("q_act", XTv[:, :, t0:t0 + 128], st[:, :, :], reads=[st], writes=[T["XT_b"]])
    C.release(m)


def load_vec_fm(C, dst_buf, dst_ap, src_ap, q="q_sp"):
    C.P.dma(q, dst_ap, src_ap.rearrange("(j p) -> p j", p=128), writes=[dst_buf], allow_slow_non_contiguous=True)


def phase_mod(C, T, l):
    P = C.P
    MOD, A1, A2 = T["MOD"], T["A1"], T["A2"]
    m = C.mark()
    cv = C.alloc("cv", (8, 2), F32)
    sc = C.alloc("sc", (8, 2), F32)
    bm = C.alloc("bm", (48,), F32)
    gn = C.alloc("gn", (2, 8), F32)
    wm = [C.alloc("wm%d" % i, (8, 512), F32) for i in range(2)]
    P.dma("q_sp", cv[:, :, 0], T["c"].rearrange("(j p) -> p j", p=128), writes=[cv], allow_slow_non_contiguous=True)
    P.dma("q_sp", cv[:, :, 1], T["c_ctx"].rearrange("(j p) -> p j", p=128), writes=[cv], allow_slow_non_contiguous=True)
    load_vec_fm(C, bm, bm[:, :], T["b_mod"][l])
    load_vec_fm(C, gn, gn[:, 0, :], T["norm1_g"][l])
    load_vec_fm(C, gn, gn[:, 1, :], T["norm2_g"][l])
    C.act(sc, sc[:, :, :], cv[:, :, :], AF.Silu, [cv])
    wv = T["w_mod"][l].rearrange("(k p) c -> p k c", p=128)
    for grp in range(12):
        w = C.rot("wm", wm)
        P.dma("q_sp" if grp % 2 == 0 else "q_act", w[:, :, :], wv[:, :, grp * 512:(grp + 1) * 512], writes=[w])
        ps = C.rot("ps_mod", C.ps[0:2])
        for jj in range(4):
            for k in range(8):
                C.mm(ps, ps[:, jj * 2:jj * 2 + 2], w[:, k, jj * 128:(jj + 1) * 128], sc[:, k, :], k == 0, k == 7, [w, sc])
        for jj in range(4):
            j = grp * 4 + jj
            C.ts("dve", MOD, MOD[:, j, :], ps[:, jj * 2:jj * 2 + 2], bm[:, j:j + 1], ALU.add, [ps, bm])
    for who in range(2):
        C.stt(A1, A1[:, :, who], MOD[:, 8:16, who], 1.0, gn[:, 0, :], ALU.add, ALU.mult, [MOD, gn])
        C.stt(A2, A2[:, :, who], MOD[:, 32:40, who], 1.0, gn[:, 1, :], ALU.add, ALU.mult, [MOD, gn])
    C.release(m)


def norm_modulate(C, T, xt, n, who, A, shift_chunk, outs, tmp, sq, rstd, ps_ss):
    MOD = T["MOD"]
    C.act(sq, sq[:, :, 0:n], xt[:, :, 0:n], AF.Square, [xt])
    for k in range(8):
        C.mm(ps_ss, ps_ss[:, 0:n], T["onesF"][:, :], sq[:, k, 0:n], k == 0, k == 7, [sq, T["onesF_b"]])
    C.act(rstd, rstd[:, 0:n], ps_ss[:, 0:n], AF.Sqrt, [ps_ss], scale=1.0 / D, bias=T["eps"][:, 0:1])
    C.recip(rstd, rstd[:, 0:n], rstd[:, 0:n], [rstd])
    for k in range(8):
        C.tt("dve", tmp, tmp[:, k, 0:n], xt[:, k, 0:n], rstd[:, 0:n], ALU.mult, [xt, rstd])
        for (ob, fn) in outs:
            C.act(ob, fn(k), tmp[:, k, 0:n], AF.Identity, [tmp, A, MOD],
                  scale=A[:, k, who:who + 1], bias=MOD[:, shift_chunk * 8 + k, who:who + 1])


def phase_inproj(C, T, l):
    P = C.P
    m = C.mark()
    HT = C.alloc("HT", (8, LT), BF16)
    XTv = T["XT"].rearrange("(k p) t -> p k t", p=128)
    m1 = C.mark()
    xt = [C.alloc("xt%d" % i, (8, 512), F32) for i in range(2)]
    tmp = C.alloc("tmp", (8, 512), F32)
    sq = C.alloc("sq", (8, 512), F32)
    rstd = C.alloc("rstd", (512,), F32)
    for (t0, n, who) in TOK_TILES:
        x_ = C.rot("xt", xt)
        P.dma("q_sp", x_[:, :, 0:n], XTv[:, :, t0:t0 + n], reads=[T["XT_b"]], writes=[x_])
        norm_modulate(C, T, x_, n, who, T["A1"], 0, [(HT, lambda k, t0=t0, n=n: HT[:, k, t0:t0 + n])],
                      tmp, sq, rstd, C.ps[7])
    C.release(m1)
    gq = C.alloc("gq", (1,), F32)
    gk = C.alloc("gk", (1,), F32)
    for h in range(2):
        P.dma("q_sp", gq[h * 64:(h + 1) * 64, :], T["q_norm_g"][l].rearrange("(p o) -> p o", o=1), writes=[gq],
              allow_slow_non_contiguous=True)
        P.dma("q_sp", gk[h * 64:(h + 1) * 64, :], T["k_norm_g"][l].rearrange("(p o) -> p o", o=1), writes=[gk],
              allow_slow_non_contiguous=True)
    cosT = C.alloc("cosT", (LT,), BF16)
    sinT = C.alloc("sinT", (LT,), BF16)
    P.dma("q_sp", cosT[:, :], T["cos"], writes=[cosT])
    P.dma("q_sp", sinT[:, :], T["sin"], writes=[sinT])
    wt = [C.alloc("wt%d" % i, (8, 512), BF16) for i in range(2)]
    stg = [C.alloc("stg%d" % i, (512,), BF16) for i in range(4)]
    sqh = C.alloc("sqh", (512,), F32)
    rs = C.alloc("rs", (512,), F32)
    qn = C.alloc("qn", (512,), BF16)
    t1 = C.alloc("t1", (512,), F32)
    wv_ = T["w_in"][l].rearrange("(k p) c -> p k c", p=128)
    dests = {}
    for cb in range(46):
        if cb < 4:
            dests[cb] = ("copy", T["UT"], T["UT_b"], cb)
        elif cb == 4:
            dests[cb] = ("rope", T["KGT"], T["KGT_b"], 0, gk)
        elif cb == 5:
            dests[cb] = None
        elif cb < 10:
            dests[cb] = ("copy", T["KNT"], T["KNT_b"], cb - 6)
        elif cb < 14:
            dests[cb] = None
        elif cb < 18:
            dests[cb] = ("rope", T["QGT"], T["QGT_b"], cb - 14, gq)
        elif cb < 22:
            dests[cb] = ("copy", T["QNT"], T["QNT_b"], cb - 18)
        else:
            dests[cb] = ("sig", T["GT"], T["GT_b"], cb - 22)
    groups = [(g * 512, 512) for g in range(11)] + [(5632, 256)]
    for (c0, ncols) in groups:
        w = C.rot("wt", wt)
        P.dma("q_pool", w[:, :, 0:ncols], wv_[:, :, c0:c0 + ncols], writes=[w])
        for bl in range(ncols // 128):
            cb = c0 // 128 + bl
            dd = dests[cb]
            if dd is None:
                continue
            for (t0, n, who) in TOK_TILES:
                ps = C.rot("ps_in", C.ps[0:3])
                for k in range(8):
                    C.mm(ps, ps[:, 0:n], w[:, k, bl * 128:(bl + 1) * 128], HT[:, k, t0:t0 + n], k == 0, k == 7, [w, HT])
                st = C.rot("stg", stg)
                kind, dt_, db, blk = dd[0], dd[1], dd[2], dd[3]
                if kind == "copy":
                    C.copy("act", st, st[:, 0:n], ps[:, 0:n], [ps])
                elif kind == "sig":
                    C.act(st, st[:, 0:n], ps[:, 0:n], AF.Sigmoid, [ps])
                else:
                    gvec = dd[4]
                    pss, psr = C.ps[3], C.ps[4]
                    C.act(sqh, sqh[:, 0:n], ps[:, 0:n], AF.Square, [ps])
                    C.mm(pss, pss[:, 0:n], T["blk64F"][:, :], sqh[:, 0:n], True, True, [sqh, T["blk64F_b"]])
                    C.act(rs, rs[:, 0:n], pss[:, 0:n], AF.Sqrt, [pss], scale=1.0 / 64, bias=T["eps"][:, 0:1])
                    C.recip(rs, rs[:, 0:n], rs[:, 0:n], [rs])
                    C.stt(qn, qn[:, 0:n], ps[:, 0:n], gvec[:, 0:1], rs[:, 0:n], ALU.mult, ALU.mult, [ps, gvec, rs])
                    C.mm(psr, psr[:, 0:n], T["rotM"][:, :], qn[:, 0:n], True, True, [qn, T["rotM_b"]])
                    C.tt("dve", t1, t1[:, 0:n], qn[:, 0:n], cosT[:, t0:t0 + n], ALU.mult, [qn, cosT])
                    C.tt("dve", rs, rs[:, 0:n], psr[:, 0:n], sinT[:, t0:t0 + n], ALU.mult, [psr, sinT])
                    C.tt("dve", st, st[:, 0:n], t1[:, 0:n], rs[:, 0:n], ALU.add, [t1, rs])
                P.dma("q_sp", dt_[blk * 128:(blk + 1) * 128, t0:t0 + n], st[:, 0:n], reads=[st], writes=[db])
    wvv = C.alloc("wvv", (8, 640), BF16)
    P.dma("q_pool", wvv[:, :, 0:128], wv_[:, :, 640:768], writes=[wvv])
    P.dma("q_pool", wvv[:, :, 128:640], wv_[:, :, 1280:1792], writes=[wvv])
    vst = [C.alloc("vst%d" % i, (640,), BF16) for i in range(2)]
    for tt in range(LT // 128):
        t0 = tt * 128
        pa = C.rot("ps_va", C.ps[0:2])
        pb = C.rot("ps_vb", C.ps[2:4])
        for k in range(8):
            C.mm(pa, pa[:, 0:128], HT[:, k, t0:t0 + 128], wvv[:, k, 0:128], k == 0, k == 7, [HT, wvv])
        for k in range(8):
            C.mm(pb, pb[:, 0:512], HT[:, k, t0:t0 + 128], wvv[:, k, 128:640], k == 0, k == 7, [HT, wvv])
        vs = C.rot("vst", vst)
        C.copy("act", vs, vs[:, 0:128], pa[:, 0:128], [pa])
        C.copy("dve", vs, vs[:, 128:640], pb[:, 0:512], [pb])
        P.dma("q_sp", T["VG"][t0:t0 + 128, :], vs[:, 0:128], reads=[vs], writes=[T["VG_b"]])
        P.dma("q_act", T["VN"][t0:t0 + 128, :], vs[:, 128:640], reads=[vs], writes=[T["VN_b"]])
    C.release(m)


def _attn_finish(C, T, OD, n, rcb, bcs, stg, psB, dst_ap, dst_b):
    C.recip(rcb, rcb[64:65, 0:n], OD[64:65, 0:n], [OD])
    C.mm(psB, psB[0:64, 0:n], T["onesF"][64:65, 0:64], rcb[64:65, 0:n], True, True, [T["onesF_b"], rcb])
    C.copy("act", bcs, bcs[0:64, 0:n], psB[0:64, 0:n], [psB])
    st = C.rot("afst", stg)
    C.tt("dve", st, st[0:64, 0:n], OD[0:64, 0:n], bcs[0:64, 0:n], ALU.mult, [OD, bcs])
    C.P.dma("q_act", dst_ap, st[0:64, 0:n], reads=[st], writes=[dst_b])


def phase_gqa(C, T, l, need_ctx):
    P = C.P
    m = C.mark()
    LA = 1
    KK = [C.alloc("KK%d" % kv, (LT,), BF16) for kv in range(2)]
    for kv in range(2):
        for half in range(2):
            P.dma("q_sp" if half == 0 else "q_act", KK[kv][half * 64:(half + 1) * 64, :],
                  T["KGT"][kv * 64:(kv + 1) * 64, :], reads=[T["KGT_b"]], writes=[KK[kv]])
    V = C.alloc("Vg", (34, 2, 65), BF16)
    for kv in range(2):
        P.dma("q_sp", V[:, :, kv, 0:64], T["VG"].rearrange("(t p) c -> p t c", p=128)[:, :, kv * 64:(kv + 1) * 64],
              reads=[T["VG_b"]], writes=[V])
    C.memset("dve", V, V[:, :, :, 64], 1.0)
    qb = [C.alloc("qb%d" % i, (512,), BF16) for i in range(2)]
    pts = [C.alloc("pt%d" % i, (512,), BF16) for i in range(4)]
    rcb = C.alloc("rcb", (512,), F32)
    bcs = C.alloc("bcs", (512,), F32)
    stg = [C.alloc("gst%d" % i, (512,), BF16) for i in range(2)]
    tiles = TOK_TILES if need_ctx else TOK_TILES[:8]
    items = []
    for (t0, n, who) in tiles:
        kts = list(range(34)) if who == 0 else [32, 33]
        for j in range(4):
            for ki, kt in enumerate(kts):
                items.append((t0, n, j, ki, kt, len(kts)))
    state = {}
    for idx in range(len(items) + LA):
        if idx < len(items):
            (t0, n, j, ki, kt, nk) = items[idx]
            if ki == 0:
                q = C.rot("qb", qb)
                P.dma("q_sp", q[:, 0:n], T["QGT"][j * 128:(j + 1) * 128, t0:t0 + n], reads=[T["QGT_b"]], writes=[q])
                state["q"] = q
            q = state["q"]
            kv = (2 * j) // 4
            Ss = [C.rot("psS", C.ps[0:4]) for p in range(2)]
            for p in range(2):
                C.mm(Ss[p], Ss[p][:, 0:n], KK[kv][p * 64:(p + 1) * 64, kt * 128:(kt + 1) * 128], q[p * 64:(p + 1) * 64, 0:n],
                     True, True, [KK[kv], q])
            ptp = []
            for p in range(2):
                pt = C.rot("pts", pts)
                C.act(pt, pt[:, 0:n], Ss[p][:, 0:n], AF.Exp, [Ss[p]], scale=0.125)
                ptp.append(pt)
            state[idx] = ptp
        if idx >= LA:
            (t0, n, j, ki, kt, nk) = items[idx - LA]
            ptp = state.pop(idx - LA)
            kv = (2 * j) // 4
            if ki == 0:
                state["OD"] = [C.rot("psO", C.ps[4:8]) for p in range(2)]
            ODs = state["OD"]
            for p in range(2):
                C.mm(ODs[p], ODs[p][0:65, 0:n], V[:, kt, kv, :], ptp[p][:, 0:n], ki == 0, ki == nk - 1, [V, ptp[p]])
            if ki == nk - 1:
                for p in range(2):
                    h = 2 * j + p
                    _attn_finish(C, T, ODs[p], n, rcb, bcs, stg, C.rot("psS", C.ps[0:4]),
                                 T["YGT"][h * 64:(h + 1) * 64, t0:t0 + n], T["YGT_b"])
    C.release(m)


def phase_na(C, T, l, need_ctx):
    P = C.P
    m = C.mark()
    LA = 1
    BT = C.alloc("BT", (8, 14, 64), BF16)
    m1 = C.mark()
    bst = C.alloc("bst", (8, 14, 64), F32)
    msk = C.alloc("msk", (8, 14, 64), BF16)
    for il in range(2):
        for h in range(8):
            P.dma("q_sp" if h % 2 == 0 else "q_act", bst[il * 64:(il + 1) * 64, h, :, :],
                  T["rpbT"][l, h, il:il + 14, :, :].rearrange("r k q -> k r q"), writes=[bst])
    P.dma("q_act", msk[:, :, :, :], T["namask"], writes=[msk])
    C.stt(BT, BT[:, :, :, :].rearrange("p a b c -> p (a b c)"), bst[:, :, :, :].rearrange("p a b c -> p (a b c)"), 8.0,
          msk[:, :, :, :].rearrange("p a b c -> p (a b c)"), ALU.mult, ALU.add, [bst, msk])
    C.release(m1)
    KT = C.alloc("KTn", (4, LT), BF16)
    QT = C.alloc("QTn", (4, LT), BF16)
    V1 = C.alloc("V1", (34, 8, 65), BF16)
    V2 = C.alloc("V2", (33, 8, 65), BF16)
    P.dma("q_sp", KT[:, :, :], T["KNT"].rearrange("(j p) t -> p j t", p=128), reads=[T["KNT_b"]], writes=[KT])
    P.dma("q_act", QT[:, :, :], T["QNT"].rearrange("(j p) t -> p j t", p=128), reads=[T["QNT_b"]], writes=[QT])
    for h in range(8):
        P.dma("q_sp", V1[:, :, h, 0:64], T["VN"].rearrange("(t p) c -> p t c", p=128)[:, :, h * 64:(h + 1) * 64],
              reads=[T["VN_b"]], writes=[V1])
        P.dma("q_act", V2[:, :, h, 0:64], T["VN"][64:64 + 33 * 128, :].rearrange("(t p) c -> p t c", p=128)[:, :, h * 64:(h + 1) * 64],
              reads=[T["VN_b"]], writes=[V2])
    for h in range(8):
        C.memset("dve", V1, V1[:, :, h, 64], 1.0)
        C.memset("pool", V2, V2[:, :, h, 64], 1.0)
    identB = C.alloc("identB", (128,), BF16)
    C.copy("dve", identB, identB[:, :], T["identF"][:, :], [T["identF_b"]])
    pts = [C.alloc("npt%d" % i, (512,), BF16) for i in range(4)]
    rcb = C.alloc("nrcb", (512,), F32)
    bcs = C.alloc("nbcs", (512,), F32)
    stg = [C.alloc("nst%d" % i, (512,), BF16) for i in range(2)]
    items = []
    for j in range(4):
        for rb in range(8):
            for rl in range(8):
                items.append(("lat", j, rb, rl))
        if need_ctx:
            items.append(("ctx", j, 0, 0))
    state = {}
    for idx in range(len(items) + LA):
        if idx < len(items):
            (kind, j, rb, rl) = items[idx]
            Ss = [C.rot("psS", C.ps[0:4]) for p in range(2)]
            ptp = [C.rot("npts", pts) for p in range(2)]
            hs = [slice(p * 64, (p + 1) * 64) for p in range(2)]
            if kind == "lat":
                r = rb * 8 + rl
                rs_ = min(max(r - 4, 0), 56)
                kb = rs_ * 64
                for c in range(4):
                    pi = 2 * c + 7 - (r - rs_)
                    for p in range(2):
                        C.mm(Ss[p], Ss[p][:, c * 64:(c + 1) * 64], KT[hs[p], j, kb + c * 128:kb + (c + 1) * 128],
                             QT[hs[p], j, r * 64:(r + 1) * 64], True, False, [KT, QT])
                    for p in range(2):
                        C.mm(Ss[p], Ss[p][:, c * 64:(c + 1) * 64], identB[:, :], BT[:, 2 * j + p, pi, :], False, True, [identB, BT])
                for cc in range(2):
                    for p in range(2):
                        C.mm(Ss[p], Ss[p][:, 256 + cc * 64:256 + (cc + 1) * 64], KT[hs[p], j, NL + cc * 128:NL + (cc + 1) * 128],
                             QT[hs[p], j, r * 64:(r + 1) * 64], True, True, [KT, QT])
                for p in range(2):
                    C.act(ptp[p], ptp[p][:, 0:384], Ss[p][:, 0:384], AF.Exp, [Ss[p]], scale=0.125)
            else:
                for cc in range(2):
                    for p in range(2):
                        C.mm(Ss[p], Ss[p][:, cc * 256:(cc + 1) * 256], KT[hs[p], j, NL + cc * 128:NL + (cc + 1) * 128],
                             QT[hs[p], j, NL:NL + 256], True, True, [KT, QT])
                for p in range(2):
                    C.act(ptp[p], ptp[p][:, 0:512], Ss[p][:, 0:512], AF.Exp, [Ss[p]], scale=0.125)
            state[idx] = ptp
        if idx >= LA:
            (kind, j, rb, rl) = items[idx - LA]
            ptp = state.pop(idx - LA)
            if rl == 0:
                state["OD"] = [C.rot("psO", C.ps[4:8]) for p in range(2)]
            ODs = state["OD"]
            if kind == "lat":
                r = rb * 8 + rl
                rs_ = min(max(r - 4, 0), 56)
                for c in range(6):
                    for p in range(2):
                        h = 2 * j + p
                        if c < 4:
                            vt = V1[:, rs_ // 2 + c, h, :] if rs_ % 2 == 0 else V2[:, (rs_ - 1) // 2 + c, h, :]
                        else:
                            vt = V1[:, 32 + (c - 4), h, :]
                        C.mm(ODs[p], ODs[p][0:65, rl * 64:(rl + 1) * 64], vt, ptp[p][:, c * 64:(c + 1) * 64], c == 0, c == 5,
                             [V1, V2, ptp[p]])
                if rl == 7:
                    for p in range(2):
                        h = 2 * j + p
                        _attn_finish(C, T, ODs[p], 512, rcb, bcs, stg, C.rot("psS", C.ps[0:4]),
                                     T["YNT"][h * 64:(h + 1) * 64, rb * 512:(rb + 1) * 512], T["YNT_b"])
            else:
                for cc in range(2):
                    for p in range(2):
                        h = 2 * j + p
                        C.mm(ODs[p], ODs[p][0:65, 0:256], V1[:, 32 + cc, h, :], ptp[p][:, cc * 256:(cc + 1) * 256], cc == 0, cc == 1,
                             [V1, ptp[p]])
                for p in range(2):
                    h = 2 * j + p
                    _attn_finish(C, T, ODs[p], 256, rcb, bcs, stg, C.rot("psS", C.ps[0:4]),
                                 T["YNT"][h * 64:(h + 1) * 64, NL:NL + 256], T["YNT_b"])
    C.release(m)


def phase_ssm(C, T, l, need_ctx):
    P = C.P
    m = C.mark()
    TT = 128
    NCH = LT // TT
    uT = C.alloc("uT", (4, LT), BF16)
    P.dma("q_sp", uT[:, :, :], T["UT"].rearrange("(k p) t -> p k t", p=128), reads=[T["UT_b"]], writes=[uT])
    Yacc = C.alloc("Yacc", (4, LT), BF16)
    dvec = C.alloc("dvec", (4,), F32)
    load_vec_fm(C, dvec, dvec[:, :], T["ssm_d"][l])
    halfpi = C.alloc("halfpi", (1,), F32)
    C.memset("dve", halfpi, halfpi[:, :], math.pi / 2)
    rm = C.alloc("rm", (16,), F32)
    P.dma("q_sp", rm[:, :], T["rowmask"], writes=[rm])
    identF = T["identF"]
    for d in (1, 0):
        first = (d == 1)
        m2 = C.mark()
        sm = lambda nm, n=16: C.alloc(nm, (n,), F32)
        ldt, are, aim, dt, rmag, th, s_, c_, t_, s2 = [sm(x) for x in ("ldt", "are", "aim", "dt", "rmag", "th", "s_", "c_", "t_", "s2")]
        Wre = C.alloc("Wre", (8, 16), F32)
        Wim = C.alloc("Wim", (8, 16), F32)
        abr, abi, nr, den, kr, ki, nki, ta, tb = [sm(x) for x in ("abr", "abi", "nr", "den", "kr", "ki", "nki", "ta", "tb")]
        for gl in range(2):
            sl = slice(gl * 64, (gl + 1) * 64)
            P.dma("q_sp", ldt[sl, :], T["ssm_log_dt"][l, d].rearrange("(cg gl) -> gl cg", gl=2)[gl:gl + 1, :].partition_broadcast(64),
                  writes=[ldt], allow_slow_non_contiguous=True)
            P.dma("q_sp", are[sl, :], T["ssm_a_re"][l, d].rearrange("(cg gl) p -> gl p cg", gl=2)[gl], writes=[are],
                  allow_slow_non_contiguous=True)
            P.dma("q_act", aim[sl, :], T["ssm_a_im"][l, d].rearrange("(cg gl) p -> gl p cg", gl=2)[gl], writes=[aim],
                  allow_slow_non_contiguous=True)
        C.act(dt, dt[:, :], ldt[:, :], AF.Exp, [ldt])
        C.tt("dve", ta, ta[:, :], are[:, :], dt[:, :], ALU.mult, [are, dt])
        C.act(rmag, rmag[:, :], ta[:, :], AF.Exp, [ta])
        C.tt("dve", th, th[:, :], aim[:, :], dt[:, :], ALU.mult, [aim, dt])
        C.act(s_, s_[:, :], th[:, :], AF.Sin, [th], scale=1.0 / 16)
        C.act(c_, c_[:, :], th[:, :], AF.Sin, [th, halfpi], scale=1.0 / 16, bias=halfpi[:, 0:1])
        for _ in range(4):
            C.tt("dve", t_, t_[:, :], s_[:, :], c_[:, :], ALU.mult, [s_, c_])
            C.tt("dve", s2, s2[:, :], s_[:, :], s_[:, :], ALU.mult, [s_])
            C.ts("dve", s_, s_[:, :], t_[:, :], 2.0, ALU.mult, [t_])
            C.ts("dve", c_, c_[:, :], s2[:, :], -2.0, ALU.mult, [s2], s2=1.0, op1=ALU.add)
        C.copy("dve", Wre, Wre[:, 0, :], c_[:, :], [c_])
        C.copy("dve", Wim, Wim[:, 0, :], s_[:, :], [s_])
        for k in range(7):
            C.tt("dve", ta, ta[:, :], Wre[:, k, :], Wre[:, k, :], ALU.mult, [Wre])
            C.tt("dve", tb, tb[:, :], Wim[:, k, :], Wim[:, k, :], ALU.mult, [Wim])
            C.tt("dve", Wre, Wre[:, k + 1, :], ta[:, :], tb[:, :], ALU.subtract, [ta, tb])
            C.tt("dve", ta, ta[:, :], Wre[:, k, :], Wim[:, k, :], ALU.mult, [Wre, Wim])
            C.ts("dve", Wim, Wim[:, k + 1, :], ta[:, :], 2.0, ALU.mult, [ta])
        C.tt("dve", abr, abr[:, :], rmag[:, :], c_[:, :], ALU.mult, [rmag, c_])
        C.tt("dve", abi, abi[:, :], rmag[:, :], s_[:, :], ALU.mult, [rmag, s_])
        C.ts("dve", nr, nr[:, :], abr[:, :], -1.0, ALU.add, [abr])
        C.tt("dve", ta, ta[:, :], are[:, :], are[:, :], ALU.mult, [are])
        C.tt("dve", tb, tb[:, :], aim[:, :], aim[:, :], ALU.mult, [aim])
        C.tt("dve", den, den[:, :], ta[:, :], tb[:, :], ALU.add, [ta, tb])
        C.recip(den, den[:, :], den[:, :], [den])
        C.tt("dve", ta, ta[:, :], nr[:, :], are[:, :], ALU.mult, [nr, are])
        C.tt("dve", tb, tb[:, :], abi[:, :], aim[:, :], ALU.mult, [abi, aim])
        C.tt("dve", ta, ta[:, :], ta[:, :], tb[:, :], ALU.add, [ta, tb])
        C.tt("dve", kr, kr[:, :], ta[:, :], den[:, :], ALU.mult, [ta, den])
        C.tt("dve", ta, ta[:, :], abi[:, :], are[:, :], ALU.mult, [abi, are])
        C.tt("dve", tb, tb[:, :], nr[:, :], aim[:, :], ALU.mult, [nr, aim])
        C.tt("dve", ta, ta[:, :], ta[:, :], tb[:, :], ALU.subtract, [ta, tb])
        C.tt("dve", ki, ki[:, :], ta[:, :], den[:, :], ALU.mult, [ta, den])
        C.ts("dve", nki, nki[:, :], ki[:, :], -1.0, ALU.mult, [ki])
        WB = C.alloc("WB", (16, 2, 128), BF16)
        WC = C.alloc("WC", (16, 2, 128), BF16)
        if d == 0:
            for nm, bf in (("rmag", rmag), ("th", th), ("kr", kr), ("ki", ki), ("c_", c_), ("s_", s_)):
                dump(C, T, nm, bf[:, :], bf)
            dump(C, T, "Wre", Wre[:, :, :], Wre)
        m3 = C.mark()
        bre = C.alloc("bre", (16, 16), F32)
        bim = C.alloc("bim", (16, 16), F32)
        for gl in range(2):
            sl = slice(gl * 64, (gl + 1) * 64)
            P.dma("q_sp", bre[sl, :, :], T["ssm_b_re"][l, d].rearrange("(cg gl) p h -> gl p cg h", gl=2)[gl], writes=[bre])
            P.dma("q_act", bim[sl, :, :], T["ssm_b_im"][l, d].rearrange("(cg gl) p h -> gl p cg h", gl=2)[gl], writes=[bim])
        Bxr = C.alloc("Bxr", (16, 128), F32)
        Bxi = C.alloc("Bxi", (16, 128), F32)
        C.memset("pool", Bxr, Bxr[:, :, :], 0.0)
        C.memset("pool", Bxi, Bxi[:, :, :], 0.0)
        tq = C.alloc("tq", (16,), F32)
        for cg in range(16):
            for gl in range(2):
                sl = slice(gl * 64, (gl + 1) * 64)
                off = ((2 * cg + gl) % 8) * 16
                C.ts("dve", tq, tq[sl, :], bre[sl, cg, :], kr[sl, cg:cg + 1], ALU.mult, [bre, kr])
                C.stt(Bxr, Bxr[sl, cg, off:off + 16], bim[sl, cg, :], nki[sl, cg:cg + 1], tq[sl, :], ALU.mult, ALU.add, [bim, nki, tq])
                C.ts("dve", tq, tq[sl, :], bim[sl, cg, :], kr[sl, cg:cg + 1], ALU.mult, [bim, kr])
                C.stt(Bxi, Bxi[sl, cg, off:off + 16], bre[sl, cg, :], ki[sl, cg:cg + 1], tq[sl, :], ALU.mult, ALU.add, [bre, ki, tq])
        for cg in range(16):
            ps = C.rot("ps_w", C.ps[0:4])
            C.mm(ps, ps[:, 0:128], Bxr[:, cg, :], identF[:, :], True, True, [Bxr, identF])
            C.mm(ps, ps[:, 128:256], Bxi[:, cg, :], identF[:, :], True, True, [Bxi, identF])
            C.copy("act", WB, WB[:, cg, :, :], ps[:, 0:256].rearrange("p (a b) -> p a b", a=2), [ps])
        Cn = [C.alloc("Cnre", (4, 64), F32), C.alloc("Cnim", (4, 64), F32)]
        P.dma("q_sp", Cn[0][:, :, :], T["ssm_c_re"][l, d].rearrange("(a g) h p -> (g h) a p", a=4), writes=[Cn[0]])
        P.dma("q_act", Cn[1][:, :, :], T["ssm_c_im"][l, d].rearrange("(a g) h p -> (g h) a p", a=4), writes=[Cn[1]])
        Xs = [C.alloc("Xs%d" % i, (128,), F32) for i in range(4)]
        for cg in range(16):
            ps = C.rot("ps_w", C.ps[0:4])
            for x in range(2):
                Xt = C.rot("Xs", Xs)
                for gl in range(2):
                    k = (2 * cg + gl) % 8 + (8 if x == 1 else 0)
                    C.ts("dve", Xt, Xt[:, gl * 64:(gl + 1) * 64], Cn[x][:, cg // 4, :], rm[:, k:k + 1], ALU.mult, [Cn[x], rm])
                C.mm(ps, ps[:, x * 128:(x + 1) * 128], Xt[:, :], identF[:, :], True, True, [Xt, identF])
            C.copy("act", WC, WC[:, cg, :, :], ps[:, 0:256].rearrange("p (a b) -> p a b", a=2), [ps])
        if d == 0:
            dump(C, T, "WB", WB[:, :, :, :].rearrange("p a b c -> p (a b c)"), WB, BF16)
            dump(C, T, "WC", WC[:, :, :, :].rearrange("p a b c -> p (a b c)"), WC, BF16)
        C.release(m3)
        cosT = C.alloc("scos", (16, TT), F32)
        sinT = C.alloc("ssin", (16, TT), F32)
        m4 = C.mark()
        tA = C.alloc("tA", (16, 64), F32)
        tB = C.alloc("tB", (16, 64), F32)
        C.memset("dve", cosT, cosT[:, :, 0:1], 1.0)
        C.memset("dve", sinT, sinT[:, :, 0:1], 0.0)
        for k in range(7):
            st = 1 << k
            wr = Wre[:, k, :].unsqueeze(2).to_broadcast([128, 16, st])
            wi = Wim[:, k, :].unsqueeze(2).to_broadcast([128, 16, st])
            C.tt("dve", tA, tA[:, :, 0:st], cosT[:, :, 0:st], wr, ALU.mult, [cosT, Wre])
            C.tt("dve", tB, tB[:, :, 0:st], sinT[:, :, 0:st], wi, ALU.mult, [sinT, Wim])
            C.tt("dve", cosT, cosT[:, :, st:2 * st], tA[:, :, 0:st], tB[:, :, 0:st], ALU.subtract, [tA, tB])
            C.tt("dve", tA, tA[:, :, 0:st], cosT[:, :, 0:st], wi, ALU.mult, [cosT, Wim])
            C.tt("dve", tB, tB[:, :, 0:st], sinT[:, :, 0:st], wr, ALU.mult, [sinT, Wre])
            C.tt("dve", sinT, sinT[:, :, st:2 * st], tA[:, :, 0:st], tB[:, :, 0:st], ALU.add, [tA, tB])
        if d == 0:
            dump(C, T, "cosT", cosT[:, :, :].rearrange("p a b -> p (a b)"), cosT)
        C.release(m4)
        car = [sm("car_re"), sm("car_im")]
        C.memset("dve", car[0], car[0][:, :], 0.0)
        C.memset("dve", car[1], car[1][:, :], 0.0)
        zb = [[C.alloc("z%d_%d" % (i, x), (8, TT), F32) for x in range(2)] for i in range(2)]
        sb = [[C.alloc("s%d_%d" % (i, x), (8, TT), BF16) for x in range(2)] for i in range(2)]
        ca, cb_ = C.alloc("ca", (8,), F32), C.alloc("cb", (8,), F32)
        rv = (lambda ap: ap) if d == 0 else (lambda ap: ap[:, :, ::-1])
        order = ([32, 33] + list(range(32))) if d == 0 else list(range(NCH - 1, -1, -1))
        last = TT - 1 if d == 0 else 0
        tp2 = [[C.alloc("tq%d_%d" % (i, k), (4, TT), BF16) for k in range(4)] for i in range(2)]
        cb2 = [[C.alloc("cq%d_%d" % (i, x), (8, TT), BF16) for x in range(2)] for i in range(2)]
        bub = [[C.alloc("bu%d_%d" % (i, x), (4, TT), BF16) for x in range(2)] for i in range(2)]
        cosTb = C.alloc("scosb", (16, TT), BF16)
        sinTb = C.alloc("ssinb", (16, TT), BF16)
        C.copy("dve", cosTb, cosTb[:, :, :], cosT[:, :, :], [cosT])
        C.copy("dve", sinTb, sinTb[:, :, :], sinT[:, :, :], [sinT])
        pp2 = [[C.alloc("pq%d_%d" % (i, x), (8, TT), BF16) for x in range(4)] for i in range(1)]
        zq2 = [[C.alloc("zq%d_%d" % (i, x), (8, TT), BF16) for x in range(2)] for i in range(2)]
        its = [(ch, hf) for ch in order for hf in range(2)]

        def stageA(ch, hf):
            t0 = ch * TT
            psb = C.ps[0:4]
            cbc = C.rot("cb2", cb2)
            for cgl in range(8):
                cg = hf * 8 + cgl
                for x in range(2):
                    bank = psb[x * 2 + cgl // 4]
                    C.mm(bank, bank[:, (cgl % 4) * TT:(cgl % 4 + 1) * TT], WB[:, cg, x, :], uT[:, cg // 4, t0:t0 + TT], True, True,
                         [WB, uT])
            for b in range(2):
                cgs = slice(hf * 8 + b * 4, hf * 8 + b * 4 + 4)
                cl = slice(b * 4, b * 4 + 4)
                cosv = rv(cosTb[:, cgs, :])
                sinv = rv(sinTb[:, cgs, :])
                tpc = C.rot("tp2", tp2)
                bu = C.rot("bub", bub)
                C.copy("act", bu[0], bu[0][:, :, :], psb[b][:, :].rearrange("p (a b) -> p a b", a=4), [psb[b]])
                C.copy("act", bu[1], bu[1][:, :, :], psb[2 + b][:, :].rearrange("p (a b) -> p a b", a=4), [psb[2 + b]])
                bre_ = bu[0][:, :, :]
                bim_ = bu[1][:, :, :]
                C.tt("dve", tpc[0], tpc[0][:, :, :], cosv, bre_, ALU.mult, [cosTb, bu[0]])
                C.tt("dve", tpc[1], tpc[1][:, :, :], sinv, bim_, ALU.mult, [sinTb, bu[1]])
                C.tt("dve", cbc[0], cbc[0][:, cl, :], tpc[0][:, :, :], tpc[1][:, :, :], ALU.add, [tpc[0], tpc[1]])
                C.tt("dve", tpc[2], tpc[2][:, :, :], cosv, bim_, ALU.mult, [cosTb, bu[1]])
                C.tt("dve", tpc[3], tpc[3][:, :, :], sinv, bre_, ALU.mult, [sinTb, bu[0]])
                C.tt("dve", cbc[1], cbc[1][:, cl, :], tpc[2][:, :, :], tpc[3][:, :, :], ALU.subtract, [tpc[2], tpc[3]])
            z = C.rot("zb", zb)
            for cgl in range(8):
                cg = hf * 8 + cgl
                for x in range(2):
                    o_ = z[x][:, cgl, :] if d == 0 else z[x][:, cgl, ::-1]
                    i_ = cbc[x][:, cgl, :] if d == 0 else cbc[x][:, cgl, ::-1]
                    P.op("dve", lambda e, o_=o_, i_=i_, cg=cg, x=x: e.tensor_tensor_scan(
                        out=o_, data0=rmag[:, cg:cg + 1].to_broadcast([128, TT]), data1=i_, initial=car[x][:, cg:cg + 1],
                        op0=ALU.mult, op1=ALU.add), reads=[rmag, cbc[x], car[x]], writes=[z[x]])
            c8 = slice(hf * 8, hf * 8 + 8)
            zr, zi = z[0][:, :, last], z[1][:, :, last]
            C.tt("dve", ca, ca[:, :], Wre[:, 7, c8], zr, ALU.mult, [Wre, z[0]])
            C.tt("dve", cb_, cb_[:, :], Wim[:, 7, c8], zi, ALU.mult, [Wim, z[1]])
            C.tt("dve", car[0], car[0][:, c8], ca[:, :], cb_[:, :], ALU.subtract, [ca, cb_])
            C.tt("dve", ca, ca[:, :], Wre[:, 7, c8], zi, ALU.mult, [Wre, z[1]])
            C.tt("dve", cb_, cb_[:, :], Wim[:, 7, c8], zr, ALU.mult, [Wim, z[0]])
            C.tt("dve", car[1], car[1][:, c8], ca[:, :], cb_[:, :], ALU.add, [ca, cb_])
            return z

        def stageB(ch, hf, z):
            t0 = ch * TT
            c8 = slice(hf * 8, hf * 8 + 8)
            sv = C.rot("sb", sb)
            ppc = C.rot("pp2", pp2)
            c8v = rv(cosTb[:, c8, :])
            s8v = rv(sinTb[:, c8, :])
            zq = C.rot("zq2", zq2)
            C.copy("act", zq[0], zq[0][:, :, :], z[0][:, :, :], [z[0]])
            C.copy("act", zq[1], zq[1][:, :, :], z[1][:, :, :], [z[1]])
            C.tt("dve", ppc[0], ppc[0][:, :, :], c8v, zq[0][:, :, :], ALU.mult, [cosTb, zq[0]])
            C.tt("dve", ppc[1], ppc[1][:, :, :], s8v, zq[1][:, :, :], ALU.mult, [sinTb, zq[1]])
            C.tt("dve", sv[0], sv[0][:, :, :], ppc[0][:, :, :], ppc[1][:, :, :], ALU.subtract, [ppc[0], ppc[1]])
            C.tt("dve", ppc[2], ppc[2][:, :, :], s8v, zq[0][:, :, :], ALU.mult, [sinTb, zq[0]])
            C.tt("dve", ppc[3], ppc[3][:, :, :], c8v, zq[1][:, :, :], ALU.mult, [cosTb, zq[1]])
            C.tt("dve", sv[1], sv[1][:, :, :], ppc[2][:, :, :], ppc[3][:, :, :], ALU.add, [ppc[2], ppc[3]])
            for yl in range(2):
                yt = hf * 2 + yl
                py = C.rot("ps_y", C.ps[4:8])
                for cq in range(4):
                    cg = yt * 4 + cq
                    cgl = cg - hf * 8
                    C.mm(py, py[:, 0:TT], WC[:, cg, 0, :], sv[0][:, cgl, :], cq == 0, False, [WC, sv[0]])
                    C.mm(py, py[:, 0:TT], WC[:, cg, 1, :], sv[1][:, cgl, :], False, cq == 3, [WC, sv[1]])
                if first:
                    C.stt(Yacc, Yacc[:, yt, t0:t0 + TT], uT[:, yt, t0:t0 + TT], dvec[:, yt:yt + 1], py[:, 0:TT], ALU.mult, ALU.add,
                          [uT, dvec, py])
                else:
                    C.tt("dve", Yacc, Yacc[:, yt, t0:t0 + TT], Yacc[:, yt, t0:t0 + TT], py[:, 0:TT], ALU.add, [Yacc, py])

        pend = None
        for (ch, hf) in its:
            z = stageA(ch, hf)
            if pend is not None:
                stageB(*pend)
            pend = (ch, hf, z)
        stageB(*pend)
        dump(C, T, "Yacc%d" % d, Yacc[:, :, :].rearrange("p a b -> p (a b)"), Yacc, BF16)
        C.release(m2)
    Wg = C.alloc("Wglu", (4, 512), BF16)
    P.dma("q_pool", Wg[:, :, :], T["glu_w"][l].rearrange("(k p) c -> p k c", p=128), writes=[Wg])
    gb = C.alloc("glub", (4,), F32)
    load_vec_fm(C, gb, gb[:, :], T["glu_b"][l])
    y2 = C.alloc("gy2", (4, 512), F32)
    sg = C.alloc("gsg", (4, 512), F32)
    ge = [C.alloc("gge%d" % i, (4, 512), BF16) for i in range(2)]
    sg2 = C.alloc("gsg2", (512,), F32)
    yst = [C.alloc("yst%d" % i, (512,), BF16) for i in range(3)]
    tiles = TOK_TILES if need_ctx else TOK_TILES[:8]
    for (t0, n, who) in tiles:
        y = Yacc[:, :, t0:t0 + n]
        g_ = C.rot("gge", ge)
        C.tt("pool", y2, y2[:, :, 0:n], y, y, ALU.mult, [Yacc])
        C.ts("pool", y2, y2[:, :, 0:n], y2[:, :, 0:n], 0.044715, ALU.mult, [y2], s2=1.0, op1=ALU.add)
        C.tt("pool", y2, y2[:, :, 0:n], y2[:, :, 0:n], y, ALU.mult, [y2, Yacc])
        C.act(sg, sg[:, :, 0:n], y2[:, :, 0:n], AF.Sigmoid, [y2], scale=1.5957691216057308)
        C.tt("dve", g_, g_[:, :, 0:n], y, sg[:, :, 0:n], ALU.mult, [Yacc, sg])
        for ob in range(4):
            ps = C.rot("ps_g", C.ps[0:4])
            for k in range(4):
                C.mm(ps, ps[:, 0:n], Wg[:, k, ob * 128:(ob + 1) * 128], g_[:, k, 0:n], k == 0, k == 3, [Wg, g_])
            C.act(sg2, sg2[:, 0:n], ps[:, 0:n], AF.Sigmoid, [ps, gb], bias=gb[:, ob:ob + 1])
            st = C.rot("yst", yst)
            C.tt("dve", st, st[:, 0:n], g_[:, ob, 0:n], sg2[:, 0:n], ALU.mult, [g_, sg2])
            P.dma("q_sp", T["YST"][ob * 128:(ob + 1) * 128, t0:t0 + n], st[:, 0:n], reads=[st], writes=[T["YST_b"]])
    C.release(m)


def phase_merge(C, T, l, need_ctx):
    P = C.P
    MOD = T["MOD"]
    m = C.mark()
    Wb = []
    for i, nm in enumerate(("w_branch_ssm", "w_branch_gqa", "w_branch_na")):
        w = C.alloc("Wb%d" % i, (4, 1024), BF16)
        P.dma("q_pool", w[:, :, :], T[nm][l].rearrange("(k p) c -> p k c", p=128), writes=[w])
        Wb.append(w)
    Wo = C.alloc("Wo", (8, 1024), BF16)
    P.dma("q_pool", Wo[:, :, :], T["w_out"][l].rearrange("(k p) c -> p k c", p=128), writes=[Wo])
    Ys = [[C.alloc("my%d_%d" % (i, br), (4, 512), BF16) for br in range(3)] for i in range(2)]
    Gs = [C.alloc("mg%d" % i, (24, 512), BF16) for i in range(2)]
    xts = [C.alloc("mx%d" % i, (8, 512), F32) for i in range(2)]
    mT = C.alloc("mT", (8, 512), BF16)
    tA = [C.alloc("mtA%d" % i, (512,), F32) for i in range(2)]
    tB = [C.alloc("mtB%d" % i, (512,), F32) for i in range(2)]
    XTv = T["XT"].rearrange("(k p) t -> p k t", p=128)
    srcs = [T["YST"], T["YGT"], T["YNT"]]
    srcb = [T["YST_b"], T["YGT_b"], T["YNT_b"]]
    tiles = TOK_TILES if need_ctx else TOK_TILES[:8]
    for (t0, n, who) in tiles:
        Y = C.rot("mY", Ys)
        G = C.rot("mG", Gs)
        xt = C.rot("mx", xts)
        for br in range(3):
            P.dma("q_sp", Y[br][:, :, 0:n], srcs[br].rearrange("(k p) t -> p k t", p=128)[:, :, t0:t0 + n], reads=[srcb[br]],
                  writes=[Y[br]])
        P.dma("q_act", G[:, :, 0:n], T["GT"].rearrange("(k p) t -> p k t", p=128)[:, :, t0:t0 + n], reads=[T["GT_b"]], writes=[G])
        P.dma("q_sp", xt[:, :, 0:n], XTv[:, :, t0:t0 + n], reads=[T["XT_b"]], writes=[xt])
        for ob in range(8):
            a = C.rot("mtA", tA)
            for br in range(3):
                ps = C.rot("ps_m", C.ps[0:4])
                for k in range(4):
                    C.mm(ps, ps[:, 0:n], Wb[br][:, k, ob * 128:(ob + 1) * 128], Y[br][:, k, 0:n], k == 0, k == 3, [Wb[br], Y[br]])
                if br == 0:
                    C.tt("dve", a, a[:, 0:n], ps[:, 0:n], G[:, ob, 0:n], ALU.mult, [ps, G])
                else:
                    b = C.rot("mtB", tB)
                    C.tt("dve", b, b[:, 0:n], ps[:, 0:n], G[:, br * 8 + ob, 0:n], ALU.mult, [ps, G])
                    if br == 1:
                        C.tt("pool", a, a[:, 0:n], a[:, 0:n], b[:, 0:n], ALU.add, [a, b])
                    else:
                        C.tt("pool", mT, mT[:, ob, 0:n], a[:, 0:n], b[:, 0:n], ALU.add, [a, b])
        for ob in range(8):
            ps = C.rot("ps_m2", C.ps[4:8])
            for k in range(8):
                C.mm(ps, ps[:, 0:n], Wo[:, k, ob * 128:(ob + 1) * 128], mT[:, k, 0:n], k == 0, k == 7, [Wo, mT])
            C.stt(xt, xt[:, ob, 0:n], ps[:, 0:n], MOD[:, 16 + ob, who:who + 1], xt[:, ob, 0:n], ALU.mult, ALU.add, [ps, MOD, xt])
        P.dma("q_act", XTv[:, :, t0:t0 + n], xt[:, :, 0:n], reads=[xt], writes=[T["XT_b"]])
    C.release(m)


def phase_ffn(C, T, l, need_ctx):
    P = C.P
    MOD = T["MOD"]
    moe = (l % 2 == 1)
    j = l // 2
    if moe:
        experts = [(T["moe_w_gate"][j, e], T["moe_w_up"][j, e], T["moe_w_down"][j, e]) for e in range(NE)]
        chunks = [(b, 4) for b in range(0, 28, 4)]
    else:
        experts = [(T["ffn_w_gate"][j], T["ffn_w_up"][j], T["ffn_w_down"][j])]
        chunks = [(b, 4) for b in range(0, 20, 4)] + [(20, 2)]
    m = C.mark()
    h2 = C.alloc("h2", (8, 1024), BF16)
    oacc = C.alloc("oacc", (8, 1024), F32)
    wgs = [C.alloc("wg%d" % i, (8, 512), BF16) for i in range(2)]
    wus = [C.alloc("wu%d" % i, (8, 512), BF16) for i in range(2)]
    wds = [C.alloc("wd%d" % i, (4, 1024), BF16) for i in range(3)]
    xt = C.alloc("fx", (8, 256), F32)
    sq = C.alloc("fsq", (8, 256), F32)
    tmp = C.alloc("ftmp", (8, 256), F32)
    rstd = C.alloc("frstd", (256,), F32)
    actb = [C.alloc("actb%d" % i, (4, 512), BF16) for i in range(2)]
    sil = [C.alloc("sil%d" % i, (512,), F32) for i in range(2)]
    silg = [C.alloc("silg%d" % i, (512,), F32) for i in range(2)]
    if moe:
        h2f = C.alloc("h2f", (8, 256), F32)
        GB = C.alloc("GB", (8, 1024), BF16)
        rw = C.alloc("rw", (8, 8), F32)
        P.dma("q_sp", rw[:, :, :], T["router_w"][j].rearrange("(k p) e -> p k e", p=128), writes=[rw])
        sm = lambda nm, n: C.alloc(nm, (n,), F32)
        L, eq, L2, sel, ex, wv, gts = [sm(x, 8) for x in ("rL", "req", "rL2", "rsel", "rex", "rwv", "rgts")]
        m1, m2, nm1, den = [sm(x, 1) for x in ("rm1", "rm2", "rnm1", "rden")]
        gbl = [C.alloc("gbl%d" % i, (128,), F32) for i in range(2)]
    XTv = T["XT"].rearrange("(k p) t -> p k t", p=128)
    sts = [(i * 1024, 1024, 0) for i in range(4)] + ([(NL, 256, 1)] if need_ctx else [])
    for (ts0, TS, who) in sts:
        nsub = TS // 256
        for sub in range(nsub):
            t0 = ts0 + sub * 256
            cs0 = sub * 256
            P.dma("q_sp", xt[:, :, :], XTv[:, :, t0:t0 + 256], reads=[T["XT_b"]], writes=[xt])
            outs = [(h2, lambda k, cs0=cs0: h2[:, k, cs0:cs0 + 256])]
            if moe:
                outs.append((h2f, lambda k: h2f[:, k, :]))
            norm_modulate(C, T, xt, 256, who, T["A2"], 3, outs, tmp, sq, rstd, C.ps[7])
            if moe:
                for tt_ in range(2):
                    pl = C.ps[6]
                    for k in range(8):
                        C.mm(pl, pl[:, 0:8], h2f[:, k, tt_ * 128:(tt_ + 1) * 128], rw[:, k, :], k == 0, k == 7, [h2f, rw])
                    C.copy("act", L, L[:, :], pl[:, 0:8], [pl])
                    P.op("dve", lambda e: e.tensor_reduce(out=m1[:, :], in_=L[:, :], axis=AX.X, op=ALU.max), reads=[L], writes=[m1])
                    C.ts("dve", eq, eq[:, :], L[:, :], m1[:, 0:1], ALU.is_equal, [L, m1])
                    C.stt(L2, L2[:, :], eq[:, :], -1e30, L[:, :], ALU.mult, ALU.add, [eq, L])
                    P.op("dve", lambda e: e.tensor_reduce(out=m2[:, :], in_=L2[:, :], axis=AX.X, op=ALU.max), reads=[L2], writes=[m2])
                    C.ts("dve", sel, sel[:, :], L[:, :], m2[:, 0:1], ALU.is_ge, [L, m2])
                    C.ts("dve", nm1, nm1[:, :], m1[:, :], -1.0, ALU.mult, [m1])
                    C.act(ex, ex[:, :], L[:, :], AF.Exp, [L, nm1], bias=nm1[:, 0:1])
                    C.tt("dve", wv, wv[:, :], ex[:, :], sel[:, :], ALU.mult, [ex, sel])
                    P.op("dve", lambda e: e.tensor_reduce(out=den[:, :], in_=wv[:, :], axis=AX.X, op=ALU.add), reads=[wv], writes=[den])
                    C.recip(den, den[:, :], den[:, :], [den])
                    C.ts("dve", gts, gts[:, :], wv[:, :], den[:, 0:1], ALU.mult, [wv, den])
                    for e4 in range(2):
                        pg_ = C.rot("ps_gb", C.ps[4:6])
                        for ee in range(4):
                            e_ = e4 * 4 + ee
                            gb_ = C.rot("gbl", gbl)
                            C.copy("dve", gb_, gb_[:, :], gts[:, e_:e_ + 1].to_broadcast([128, 128]), [gts])
                            C.mm(pg_, pg_[:, ee * 128:(ee + 1) * 128], gb_[:, :], T["identF"][:, :], True, True, [gb_, T["identF_b"]])
                        pos = cs0 + tt_ * 128
                        C.copy("act", GB, GB[:, e4 * 4:(e4 + 1) * 4, pos:pos + 128],
                               pg_[:, :].rearrange("p (a b) -> p a b", a=4), [pg_])
        SW = min(512, TS)
        nsw = TS // SW
        pend = None

        def down(ab, wd, nb, cs, first):
            for ob in range(8):
                po = C.rot("ps_fo", C.ps[4:6])
                for hb in range(nb):
                    C.mm(po, po[:, 0:SW], wd[:, hb, ob * 128:(ob + 1) * 128], ab[:, hb, 0:SW], hb == 0, hb == nb - 1, [wd, ab])
                if first:
                    C.copy("act", oacc, oacc[:, ob, cs], po[:, 0:SW], [po])
                else:
                    C.tt("dve", oacc, oacc[:, ob, cs], oacc[:, ob, cs], po[:, 0:SW], ALU.add, [oacc, po])

        for ei, (wg_ap, wu_ap, wd_ap) in enumerate(experts):
            wgv = wg_ap.rearrange("(k p) c -> p k c", p=128)
            wuv = wu_ap.rearrange("(k p) c -> p k c", p=128)
            wdv = wd_ap.rearrange("(hb p) c -> p hb c", p=128)
            for (b0, nb) in chunks:
                wg, wu, wd = C.rot("wgs", wgs), C.rot("wus", wus), C.rot("wds", wds)
                P.dma("q_pool", wg[:, :, 0:nb * 128], wgv[:, :, b0 * 128:(b0 + nb) * 128], writes=[wg])
                P.dma("q_pool", wu[:, :, 0:nb * 128], wuv[:, :, b0 * 128:(b0 + nb) * 128], writes=[wu])
                P.dma("q_pool", wd[:, 0:nb, :], wdv[:, b0:b0 + nb, :], writes=[wd])
                first = (ei == 0 and b0 == 0)
                for sub in range(nsw):
                    cs = slice(sub * SW, (sub + 1) * SW)
                    ab = C.rot("actb", actb)
                    for hb in range(nb):
                        pg = C.rot("ps_fg", C.ps[0:2])
                        pu = C.rot("ps_fu", C.ps[2:4])
                        for k in range(8):
                            C.mm(pg, pg[:, 0:SW], wg[:, k, hb * 128:(hb + 1) * 128], h2[:, k, cs], k == 0, k == 7, [wg, h2])
                        for k in range(8):
                            C.mm(pu, pu[:, 0:SW], wu[:, k, hb * 128:(hb + 1) * 128], h2[:, k, cs], k == 0, k == 7, [wu, h2])
                        sl_ = C.rot("sil", sil)
                        C.act(sl_, sl_[:, 0:SW], pg[:, 0:SW], AF.Silu, [pg])
                        if moe:
                            sg_ = C.rot("silg", silg)
                            C.tt("dve", sg_, sg_[:, 0:SW], sl_[:, 0:SW], GB[:, ei, cs], ALU.mult, [sl_, GB])
                            C.tt("dve", ab, ab[:, hb, 0:SW], pu[:, 0:SW], sg_[:, 0:SW], ALU.mult, [pu, sg_])
                        else:
                            C.tt("dve", ab, ab[:, hb, 0:SW], pu[:, 0:SW], sl_[:, 0:SW], ALU.mult, [pu, sl_])
                    if pend is not None:
                        down(*pend)
                    pend = (ab, wd, nb, cs, first)
        down(*pend)
        for sub in range(nsub):
            t0 = ts0 + sub * 256
            cs = slice(sub * 256, (sub + 1) * 256)
            P.dma("q_sp", xt[:, :, :], XTv[:, :, t0:t0 + 256], reads=[T["XT_b"]], writes=[xt])
            for ob in range(8):
                C.stt(xt, xt[:, ob, :], oacc[:, ob, cs], MOD[:, 40 + ob, who:who + 1], xt[:, ob, :], ALU.mult, ALU.add, [oacc, MOD, xt])
            P.dma("q_act", XTv[:, :, t0:t0 + 256], xt[:, :, :], reads=[xt], writes=[T["XT_b"]])
    C.release(m)


def phase_final(C, T):
    P = C.P
    m = C.mark()
    gf = C.alloc("gf", (8,), F32)
    load_vec_fm(C, gf, gf[:, :], T["final_norm_g"])
    xts = [C.alloc("ox%d" % i, (8, 512), F32) for i in range(2)]
    sq = C.alloc("osq", (8, 512), F32)
    rstd = C.alloc("orstd", (512,), F32)
    yv = C.alloc("oy", (8, 512), F32)
    ots = [C.alloc("ot%d" % i, (1024,), F32) for i in range(2)]
    XTv = T["XT"].rearrange("(k p) t -> p k t", p=128)
    for (t0, n, who) in TOK_TILES[:8]:
        xt = C.rot("ox", xts)
        P.dma("q_sp", xt[:, :, :], XTv[:, :, t0:t0 + n], reads=[T["XT_b"]], writes=[xt])
        C.act(sq, sq[:, :, :], xt[:, :, :], AF.Square, [xt])
        pss = C.ps[7]
        for k in range(8):
            C.mm(pss, pss[:, 0:n], T["onesF"][:, :], sq[:, k, :], k == 0, k == 7, [sq, T["onesF_b"]])
        C.act(rstd, rstd[:, :], pss[:, 0:n], AF.Sqrt, [pss], scale=1.0 / D, bias=T["eps"][:, 0:1])
        C.recip(rstd, rstd[:, :], rstd[:, :], [rstd])
        for k in range(8):
            C.stt(yv, yv[:, k, :], xt[:, k, :], gf[:, k:k + 1], rstd[:, :], ALU.mult, ALU.mult, [xt, gf, rstd])
        for blk in range(n // 128):
            ot = C.rot("ot", ots)
            for half in range(2):
                ps = C.rot("ps_o", C.ps[0:4])
                for kk in range(4):
                    k = half * 4 + kk
                    C.mm(ps, ps[:, kk * 128:(kk + 1) * 128], yv[:, k, blk * 128:(blk + 1) * 128], T["identF"][:, :], True, True,
                         [yv, T["identF_b"]])
                C.copy("act" if half == 0 else "dve", ot, ot[:, half * 512:(half + 1) * 512], ps[:, :], [ps])
            P.dma("q_act", T["out"][t0 + blk * 128:t0 + (blk + 1) * 128, :], ot[:, :], reads=[ot])
    C.release(m)


def host_consts():
    cst = {}
    cst["identF"] = np.eye(128, dtype=np.float32)
    cst["onesF"] = np.ones((128, 128), np.float32)
    b = np.zeros((128, 128), np.float32)
    b[:64, :64] = 1.0
    b[64:, 64:] = 1.0
    cst["blk64F"] = b
    R = np.zeros((128, 128), np.float32)
    for h in range(2):
        for half in range(2):
            o = h * 64 + half * 32
            for j in range(16):
                R[o + j + 16, o + j] = -1.0
                R[o + j, o + j + 16] = 1.0
    cst["rotM"] = R.astype(ml_dtypes.bfloat16)
    t = np.arange(NL)
    pos = np.stack([t // GRID_W, t % GRID_W], -1).astype(np.float32)
    half = 32
    inv = (1.0 / (10000.0 ** (np.arange(0, half, 2, dtype=np.float32) / half))).astype(np.float32)
    ang = pos[:, :, None] * inv
    ang = np.concatenate([ang, ang], -1).reshape(NL, 64)
    cosv = np.concatenate([np.cos(ang).astype(np.float32), np.ones((NC_, 64), np.float32)], 0)
    sinv = np.concatenate([np.sin(ang).astype(np.float32), np.zeros((NC_, 64), np.float32)], 0)
    cst["cos"] = np.ascontiguousarray(np.concatenate([cosv.T, cosv.T], 0)).astype(ml_dtypes.bfloat16)
    cst["sin"] = np.ascontiguousarray(np.concatenate([sinv.T, sinv.T], 0)).astype(ml_dtypes.bfloat16)
    qc = np.arange(64)
    cs = np.clip(qc - 8, 0, 48)
    kc = np.arange(64)[:, None]
    valid = (kc >= cs[None, :]) & (kc < cs[None, :] + 16)
    mk = np.where(valid, 0.0, -240000.0).astype(np.float32)
    mk = np.broadcast_to(mk[None, :, None, None, :], (2, 64, 8, 14, 64)).reshape(128, 8, 14, 64)
    cst["namask"] = np.ascontiguousarray(mk).astype(ml_dtypes.bfloat16)
    rmk = np.zeros((128, 16), np.float32)
    for k in range(8):
        rmk[k * 16:(k + 1) * 16, k] = 1.0
        rmk[k * 16:(k + 1) * 16, 8 + k] = -1.0
    cst["rowmask"] = rmk
    return cst


def na_rpb_gather(rpb):
    kc = np.arange(64)[:, None]
    qc = np.arange(64)[None, :]
    idx = np.clip(kc - qc + 15, 0, 30)
    return np.ascontiguousarray(rpb[:, :, :, idx])


CONST_DT = {"identF": F32, "onesF": F32, "blk64F": F32, "rotM": BF16, "cos": BF16, "sin": BF16, "namask": BF16, "rowmask": F32}

WEIGHT_SPECS = [
    ("c_ctx", [D]), ("w_mod", [DEPTH, D, 6 * D]), ("b_mod", [DEPTH, 6 * D]), ("norm1_g", [DEPTH, D]),
    ("w_in", [DEPTH, D, IN_COLS]),
    ("ssm_a_re", [DEPTH, 2, 32, 64]), ("ssm_a_im", [DEPTH, 2, 32, 64]),
    ("ssm_b_re", [DEPTH, 2, 32, 64, 16]), ("ssm_b_im", [DEPTH, 2, 32, 64, 16]),
    ("ssm_c_re", [DEPTH, 2, 32, 16, 64]), ("ssm_c_im", [DEPTH, 2, 32, 16, 64]),
    ("ssm_log_dt", [DEPTH, 2, 32]), ("ssm_d", [DEPTH, 512]), ("glu_w", [DEPTH, 512, 512]), ("glu_b", [DEPTH, 512]),
    ("q_norm_g", [DEPTH, 64]), ("k_norm_g", [DEPTH, 64]), ("na_rpb", [DEPTH, 8, 15, 31]),
    ("w_branch_ssm", [DEPTH, 512, D]), ("w_branch_gqa", [DEPTH, 512, D]), ("w_branch_na", [DEPTH, 512, D]),
    ("w_out", [DEPTH, D, D]), ("norm2_g", [DEPTH, D]),
    ("ffn_w_gate", [1, D, FFN_DIM]), ("ffn_w_up", [1, D, FFN_DIM]), ("ffn_w_down", [1, FFN_DIM, D]),
    ("router_w", [1, D, NE]), ("moe_w_gate", [1, NE, D, EXPERT_DIM]), ("moe_w_up", [1, NE, D, EXPERT_DIM]),
    ("moe_w_down", [1, NE, EXPERT_DIM, D]), ("final_norm_g", [D]),
]


def build_program(stop=None, debug=()):
    nc = bass.Bass("TRN2", target_bir_lowering=False)
    wspecs = dict(WEIGHT_SPECS)

    class LazyT(dict):
        def __missing__(self, name):
            if name in wspecs:
                ap = nc.dram_tensor(name, wspecs[name], F32, kind="ExternalInput").ap()
                self[name] = ap
                self["_used"].append(name)
                return ap
            raise KeyError(name)

    T = LazyT()
    T["_used"] = []
    T["_debug"] = tuple(debug)
    T["x"] = nc.dram_tensor("x", [NL, D], F32, kind="ExternalInput").ap()
    T["ctx"] = nc.dram_tensor("ctx", [NC_, D], F32, kind="ExternalInput").ap()
    T["c"] = nc.dram_tensor("c", [D], F32, kind="ExternalInput").ap()
    cst = host_consts()
    cdram = {}
    for k, v in cst.items():
        cdram[k] = nc.dram_tensor("k_" + k, list(v.shape), CONST_DT[k], kind="ExternalInput").ap()
    T["out"] = nc.dram_tensor("out", [NL, D], F32, kind="ExternalOutput").ap()
    T["rpbT"] = nc.dram_tensor("rpbT", [DEPTH, 8, 15, 64, 64], F32, kind="ExternalInput").ap()
    T["namask"] = cdram["namask"]
    T["rowmask"] = cdram["rowmask"]

    def scratch(name, shape, dt):
        kind = "ExternalOutput" if name in debug else "Internal"
        T[name] = nc.dram_tensor("s_" + name, shape, dt, kind=kind).ap()
        T[name + "_b"] = Buf(name)

    scratch("XT", [D, LT], F32)
    scratch("UT", [512, LT], BF16)
    scratch("KGT", [128, LT], BF16)
    scratch("VG", [LT, 128], BF16)
    scratch("KNT", [512, LT], BF16)
    scratch("VN", [LT, 512], BF16)
    scratch("QGT", [512, LT], BF16)
    scratch("QNT", [512, LT], BF16)
    scratch("GT", [3072, LT], BF16)
    scratch("YST", [512, LT], BF16)
    scratch("YGT", [512, LT], BF16)
    scratch("YNT", [512, LT], BF16)

    C = Ctx(nc)
    P = C.P
    for k in ("identF", "onesF", "blk64F"):
        b = C.alloc(k, (128,), F32)
        P.dma("q_sp", b[:, :], cdram[k], writes=[b])
        T[k] = b
        T[k + "_b"] = b
    b = C.alloc("rotM", (128,), BF16)
    P.dma("q_sp", b[:, :], cdram["rotM"], writes=[b])
    T["rotM"] = b
    T["rotM_b"] = b
    T["cos"] = cdram["cos"]
    T["sin"] = cdram["sin"]
    T["eps"] = C.alloc("eps", (1,), F32)
    C.memset("dve", T["eps"], T["eps"][:, :], EPS)
    T["MOD"] = C.alloc("MOD", (48, 2), F32)
    T["A1"] = C.alloc("A1", (8, 2), F32)
    T["A2"] = C.alloc("A2", (8, 2), F32)

    def done():
        P.barrier()
        P.emit()
        return nc, cst, list(T["_used"])

    phase_setup(C, T)
    if stop == "setup":
        return done()
    for l in range(DEPTH):
        phase_mod(C, T, l)
        if stop == "mod%d" % l:
            return done()
        phase_inproj(C, T, l)
        if stop == "inproj%d" % l:
            return done()
        need_ctx = l < DEPTH - 1
        if "skip_gqa" not in debug:
            phase_gqa(C, T, l, need_ctx)
        if stop == "gqa%d" % l:
            return done()
        if "skip_na" not in debug:
            phase_na(C, T, l, need_ctx)
        if stop == "na%d" % l:
            return done()
        if "skip_ssm" not in debug:
            phase_ssm(C, T, l, need_ctx)
        if stop == "ssm%d" % l:
            return done()
        phase_merge(C, T, l, need_ctx)
        if stop == "merge%d" % l:
            return done()
        phase_ffn(C, T, l, need_ctx)
        if stop == "ffn%d" % l:
            return done()
    phase_final(C, T)
    return done()


def make_in_maps(inputs, cst, used, n_cores=8):
    maps = []
    rpbT = na_rpb_gather(np.asarray(inputs["na_rpb"]))
    for b in range(n_cores):
        m = {"x": np.ascontiguousarray(inputs["x"][b]), "ctx": np.ascontiguousarray(inputs["ctx"][b]),
             "c": np.ascontiguousarray(inputs["c"][b])}
        for name in used:
            m[name] = np.asarray(inputs[name])
        for k, v in cst.items():
            m["k_" + k] = v
        m["rpbT"] = rpbT
        maps.append(m)
    return maps


def kernel(**inputs):
    nc, cst, used = build_program()
    maps = make_in_maps(inputs, cst, used, 8)
    res = run_bass_kernel_spmd(nc, maps, core_ids=list(range(8)))
    return np.stack([r["out"] for r in res.results], 0).astype(np.float32)
```
